# Optimizing a Trainium2 kernel written in Bass

```python
import math
import jax
import jax.numpy as jnp
from jax import lax
import numpy as np

D_MODEL = 1024
BATCH = 1
SEQ = 16384
DEPTH = 4

GRID_W = 64
CTX_LEN = 256
N_MIXERS = 4
HEAD_DIM = 64
ROPE_THETA = 10000.0
Q_BLOCK = 128
NORM_EPS = 1e-6
NEG_INF = -1e30

DA_HEADS = D_MODEL // (2 * HEAD_DIM)
DA_V_DIM = 2 * HEAD_DIM
SWA_HEADS = D_MODEL // HEAD_DIM
SWA_KV_HEADS = 4
SWA_WINDOW = 128
GA_HEADS = D_MODEL // HEAD_DIM
GA_KV_HEADS = 4
NA_HEADS = D_MODEL // HEAD_DIM
NA_ROWS = 8
NA_COLS = 16
N_GROUPS = 4
EXPERTS_PER_GROUP = 8
N_EXPERTS = N_GROUPS * EXPERTS_PER_GROUP
TOP_K = 2
D_EXPERT = D_MODEL // 2
MOE_BLOCK = 128

kernel_name = 'hybrid_diff_swa_axial_natten_hmoe_dit'


def _rmsnorm(x, g):
    xf = x.astype(jnp.float32)
    y = xf * lax.rsqrt(jnp.mean(xf * xf, axis=-1, keepdims=True) + NORM_EPS)
    return (y * g.astype(jnp.float32)).astype(x.dtype)


def _modulate(h, shift, scale):
    return h * (1 + scale) + shift


def _axial_rope_tables(n):
    t = jnp.arange(n, dtype=jnp.int32)
    pos = jnp.stack([t // GRID_W, t % GRID_W], axis=-1).astype(jnp.float32)
    quarter = HEAD_DIM // 4
    inv_freq = 1.0 / (ROPE_THETA ** (jnp.arange(quarter, dtype=jnp.float32) / quarter))
    ang = pos[:, :, None] * inv_freq
    return jnp.cos(ang), jnp.sin(ang)


def _rope(x, cos, sin):
    b, n, hh, d = x.shape
    xr = x.astype(jnp.float32).reshape(b, n, hh, 2, 2, d // 4)
    x1, x2 = xr[..., 0, :], xr[..., 1, :]
    cs, sn = cos[None, :, None], sin[None, :, None]
    out = jnp.stack([x1 * cs - x2 * sn, x2 * cs + x1 * sn], axis=-2)
    return out.reshape(b, n, hh, d).astype(x.dtype)


def _project(t, w, splits):
    b, n, _ = t.shape
    y = t @ w
    outs, off = [], 0
    for nh, dh in splits:
        outs.append(y[..., off:off + nh * dh].reshape(b, n, nh, dh))
        off += nh * dh
    return outs


def _sweep_queries(fn, q):
    b, s = q.shape[:2]
    nb = s // Q_BLOCK
    qb = jnp.swapaxes(q.reshape((b, nb, Q_BLOCK) + q.shape[2:]), 0, 1)
    ob = lax.map(fn, qb)
    return jnp.swapaxes(ob, 0, 1).reshape((b, s) + ob.shape[3:])


def _gqa_attend(q, k, v, sink=None, mask=None):
    b, nq, hq, d = q.shape
    hkv = k.shape[2]
    grp = hq // hkv
    qg = q.reshape(b, nq, hkv, grp, d)
    s = jnp.einsum('bqhgd,bkhd->bhgqk', qg, k).astype(jnp.float32) / math.sqrt(d)
    if mask is not None:
        s = jnp.where(mask, s, NEG_INF)
    if sink is None:
        p = jax.nn.softmax(s, axis=-1)
    else:
        sk = jnp.broadcast_to(sink.astype(jnp.float32).reshape(1, hkv, grp, 1, 1), s.shape[:-1] + (1,))
        p = jax.nn.softmax(jnp.concatenate([s, sk], axis=-1), axis=-1)[..., :-1]
    o = jnp.einsum('bhgqk,bkhd->bqhgd', p.astype(v.dtype), v)
    return o.reshape(b, nq, hq, v.shape[-1])


def _diff_attend(q, k, v, lam):
    s = jnp.einsum('bqhmd,bkhmd->bhmqk', q, k).astype(jnp.float32) / math.sqrt(q.shape[-1])
    p = jax.nn.softmax(s, axis=-1)
    a = p[:, :, 0] - lam * p[:, :, 1]
    return jnp.einsum('bhqk,bkhd->bqhd', a.astype(v.dtype), v)


def _diff_mixer(h, hc, w_qkv, w_o, lam_vecs, subln_g, lam_init, cos, sin, need_ctx):
    b, s, _ = h.shape
    lv = lam_vecs.astype(jnp.float32)
    lam = jnp.exp(jnp.sum(lv[0] * lv[1])) - jnp.exp(jnp.sum(lv[2] * lv[3])) + lam_init
    splits = [(2 * DA_HEADS, HEAD_DIM), (2 * DA_HEADS, HEAD_DIM), (DA_HEADS, DA_V_DIM)]
    q, k, v = _project(h, w_qkv, splits)
    qc, kc, vc = _project(hc, w_qkv, splits)
    q = _rope(q, cos, sin).reshape(b, s, DA_HEADS, 2, HEAD_DIM)
    k = _rope(k, cos, sin).reshape(b, s, DA_HEADS, 2, HEAD_DIM)
    kc = kc.reshape(b, -1, DA_HEADS, 2, HEAD_DIM)
    k_all = jnp.concatenate([kc, k], axis=1)
    v_all = jnp.concatenate([vc, v], axis=1)

    def out_proj(o):
        o = _rmsnorm(o, subln_g) * (1.0 - lam_init)
        return o.reshape(o.shape[0], o.shape[1], -1) @ w_o

    y = out_proj(_sweep_queries(lambda qb: _diff_attend(qb, k_all, v_all, lam), q))
    if not need_ctx:
        return y, None
    qc = qc.reshape(b, -1, DA_HEADS, 2, HEAD_DIM)
    return y, out_proj(_diff_attend(qc, kc, vc, lam))


def _swa_mixer(h, hc, w_qkv, w_o, sink, cos, sin, need_ctx):
    b, s, _ = h.shape
    n_ctx = hc.shape[1]
    nb = s // Q_BLOCK
    splits = [(SWA_HEADS, HEAD_DIM), (SWA_KV_HEADS, HEAD_DIM), (SWA_KV_HEADS, HEAD_DIM)]
    q, k, v = _project(h, w_qkv, splits)
    qc, kc, vc = _project(hc, w_qkv, splits)
    q, k = _rope(q, cos, sin), _rope(k, cos, sin)

    def band(t):
        tb = t.reshape((b, nb, Q_BLOCK) + t.shape[2:])
        tp = jnp.pad(tb, [(0, 0), (1, 1)] + [(0, 0)] * (tb.ndim - 2))
        tband = jnp.concatenate([tp[:, :-2], tp[:, 1:-1], tp[:, 2:]], axis=2)
        return jnp.swapaxes(tband, 0, 1)

    kb, vb = band(k), band(v)
    qb = jnp.swapaxes(q.reshape(b, nb, Q_BLOCK, SWA_HEADS, HEAD_DIM), 0, 1)
    qi = jnp.arange(Q_BLOCK)[:, None]
    kj = jnp.arange(3 * Q_BLOCK)[None, :]
    in_window = jnp.abs(kj - Q_BLOCK - qi) <= SWA_WINDOW
    ctx_ok = jnp.ones((Q_BLOCK, n_ctx), dtype=bool)

    def block(args):
        q_blk, k_blk, v_blk, bi = args
        kpos = bi * Q_BLOCK - Q_BLOCK + kj
        valid = in_window & (kpos >= 0) & (kpos < s)
        mask = jnp.concatenate([ctx_ok, valid], axis=-1)
        k_all = jnp.concatenate([kc, k_blk], axis=1)
        v_all = jnp.concatenate([vc, v_blk], axis=1)
        return _gqa_attend(q_blk, k_all, v_all, sink, mask)

    o = lax.map(block, (qb, kb, vb, jnp.arange(nb)))
    y = jnp.swapaxes(o, 0, 1).reshape(b, s, -1) @ w_o
    if not need_ctx:
        return y, None
    return y, _gqa_attend(qc, kc, vc, sink).reshape(b, n_ctx, -1) @ w_o


def _axial_gqa_mixer(h, hc, w_qkv, w_o, qk_g, cos, sin, need_ctx):
    b, s, _ = h.shape
    n_ctx = hc.shape[1]
    splits = [(GA_HEADS, HEAD_DIM), (GA_KV_HEADS, HEAD_DIM), (GA_KV_HEADS, HEAD_DIM)]
    q, k, v = _project(h, w_qkv, splits)
    qc, kc, vc = _project(hc, w_qkv, splits)
    q, k = _rmsnorm(q, qk_g[0]), _rmsnorm(k, qk_g[1])
    qc, kc = _rmsnorm(qc, qk_g[0]), _rmsnorm(kc, qk_g[1])
    q, k = _rope(q, cos, sin), _rope(k, cos, sin)
    k_all = jnp.concatenate([kc, k], axis=1)
    v_all = jnp.concatenate([vc, v], axis=1)
    o = _sweep_queries(lambda qb: _gqa_attend(qb, k_all, v_all), q)
    y = o.reshape(b, s, -1) @ w_o
    if not need_ctx:
        return y, None
    return y, _gqa_attend(qc, kc, vc).reshape(b, n_ctx, -1) @ w_o


def _na_mixer(h, hc, w_qkv, w_o, rpb, need_ctx):
    b, s, _ = h.shape
    n_ctx = hc.shape[1]
    rows = s // GRID_W
    kh = min(NA_ROWS, rows)
    splits = [(NA_HEADS, HEAD_DIM)] * 3
    q, k, v = _project(h, w_qkv, splits)
    qc, kc, vc = _project(hc, w_qkv, splits)
    q_g = q.reshape(b, rows, GRID_W, NA_HEADS, HEAD_DIM)
    k_g = k.reshape(b, rows, GRID_W, NA_HEADS, HEAD_DIM)
    v_g = v.reshape(b, rows, GRID_W, NA_HEADS, HEAD_DIM)
    cols = np.arange(GRID_W)
    col_idx = np.clip(cols - NA_COLS // 2, 0, GRID_W - NA_COLS)[:, None] + np.arange(NA_COLS)[None, :]
    dc_idx = col_idx - cols[:, None] + (NA_COLS - 1)
    bias_c = rpb.astype(jnp.float32)[:, :, dc_idx]
    scale = 1.0 / math.sqrt(HEAD_DIM)
    n_nb = kh * NA_COLS

    def row_step(args):
        q_r, r = args
        r0 = jnp.clip(r - kh // 2, 0, rows - kh)
        k_r = lax.dynamic_slice_in_dim(k_g, r0, kh, axis=1)[:, :, col_idx]
        v_r = lax.dynamic_slice_in_dim(v_g, r0, kh, axis=1)[:, :, col_idx]
        dr_idx = r0 + jnp.arange(kh) - r + (NA_ROWS - 1)
        bias = jnp.take(bias_c, dr_idx, axis=1).transpose(0, 2, 1, 3)
        s_nb = jnp.einsum('bchd,bkcwhd->bhckw', q_r, k_r).astype(jnp.float32) * scale + bias[None]
        s_cx = jnp.einsum('bchd,bnhd->bhcn', q_r, kc).astype(jnp.float32) * scale
        logits = jnp.concatenate([s_nb.reshape(b, NA_HEADS, GRID_W, n_nb), s_cx], axis=-1)
        p = jax.nn.softmax(logits, axis=-1).astype(v.dtype)
        p_nb = p[..., :n_nb].reshape(b, NA_HEADS, GRID_W, kh, NA_COLS)
        return (jnp.einsum('bhckw,bkcwhd->bchd', p_nb, v_r)
                + jnp.einsum('bhcn,bnhd->bchd', p[..., n_nb:], vc))

    o = lax.map(row_step, (jnp.swapaxes(q_g, 0, 1), jnp.arange(rows)))
    y = jnp.swapaxes(o, 0, 1).reshape(b, s, -1) @ w_o
    if not need_ctx:
        return y, None
    return y, _gqa_attend(qc, kc, vc).reshape(b, n_ctx, -1) @ w_o


def _hier_moe(t, wr_g, br_g, wr_e, br_e, w_gate, w_up, w_down):
    n_tok, d = t.shape
    lg = (t @ wr_g).astype(jnp.float32) + br_g.astype(jnp.float32)
    pg = jax.nn.softmax(lg, axis=-1)
    g = jnp.argmax(lg, axis=-1)
    gate_g = jnp.take_along_axis(pg, g[:, None], axis=1)
    le = ((t @ wr_e).astype(jnp.float32) + br_e.astype(jnp.float32)).reshape(n_tok, N_GROUPS, EXPERTS_PER_GROUP)
    le_g = jnp.take_along_axis(le, g[:, None, None], axis=1)[:, 0]
    top_v, top_i = lax.top_k(le_g, TOP_K)
    weights = (gate_g * jax.nn.softmax(top_v, axis=-1)).reshape(-1)
    expert = (g[:, None] * EXPERTS_PER_GROUP + top_i).reshape(-1)
    tok = jnp.repeat(jnp.arange(n_tok, dtype=jnp.int32), TOP_K)
    n_assign = n_tok * TOP_K
    order = jnp.argsort(expert, stable=True)
    e_sorted = expert[order]
    counts = jax.ops.segment_sum(jnp.ones_like(expert), expert, num_segments=N_EXPERTS)
    padded = (counts + MOE_BLOCK - 1) // MOE_BLOCK * MOE_BLOCK
    p_end = jnp.cumsum(padded)
    p_start = p_end - padded
    start = jnp.cumsum(counts) - counts
    dest = p_start[e_sorted] + jnp.arange(n_assign) - start[e_sorted]
    cap = -(-n_assign // MOE_BLOCK) * MOE_BLOCK + N_EXPERTS * MOE_BLOCK
    n_blk = cap // MOE_BLOCK
    buf_tok = jnp.full((cap,), n_tok, jnp.int32).at[dest].set(tok[order])
    buf_w = jnp.zeros((cap,), jnp.float32).at[dest].set(weights[order])
    blk_e = jnp.clip(jnp.searchsorted(p_end, jnp.arange(n_blk) * MOE_BLOCK, side='right'), 0, N_EXPERTS - 1)
    t_pad = jnp.concatenate([t, jnp.zeros((1, d), t.dtype)], axis=0)
    xb = t_pad[buf_tok].reshape(n_blk, MOE_BLOCK, d)

    def expert_block(args):
        x_blk, e = args
        hidden = jax.nn.silu(x_blk @ w_gate[e]) * (x_blk @ w_up[e])
        return hidden @ w_down[e]

    yb = lax.map(expert_block, (xb, blk_e)).reshape(cap, d)
    out = jnp.zeros((n_tok + 1, d), t.dtype).at[buf_tok].add(yb * buf_w[:, None].astype(yb.dtype))
    return out[:n_tok]


def _n_layers_of(m):
    return len(range(m, DEPTH, N_MIXERS))


def setup_inputs(seed: int = 0) -> dict:
    key = jax.random.key(seed)
    keys = iter(jax.random.split(key, 64))

    def nrm(shape, std):
        return std * jax.random.normal(next(keys), shape, jnp.float32)

    D = D_MODEL
    n_a, n_b, n_c, n_d = (_n_layers_of(m) for m in range(N_MIXERS))
    a_cols = 4 * DA_HEADS * HEAD_DIM + DA_HEADS * DA_V_DIM
    b_cols = (SWA_HEADS + 2 * SWA_KV_HEADS) * HEAD_DIM
    c_cols = (GA_HEADS + 2 * GA_KV_HEADS) * HEAD_DIM
    d_cols = 3 * NA_HEADS * HEAD_DIM
    return {
        'x': nrm((BATCH, SEQ, D), 1.0),
        'c': nrm((BATCH, D), 1.0),
        'ctx': nrm((BATCH, CTX_LEN, D), 1.0),
        'c_ctx': nrm((D,), 1.0),
        'mod_w': nrm((DEPTH, D, 6 * D), 0.5 / math.sqrt(D)),
        'mod_b': nrm((DEPTH, 6 * D), 0.02),
        'norm1_g': 1.0 + nrm((DEPTH, D), 0.02),
        'norm2_g': 1.0 + nrm((DEPTH, D), 0.02),
        'a_w_qkv': nrm((n_a, D, a_cols), D ** -0.5),
        'a_w_o': nrm((n_a, DA_HEADS * DA_V_DIM, D), (DA_HEADS * DA_V_DIM) ** -0.5),
        'a_lam': nrm((n_a, 4, HEAD_DIM), 0.1),
        'a_subln_g': 1.0 + nrm((n_a, DA_V_DIM), 0.02),
        'b_w_qkv': nrm((n_b, D, b_cols), D ** -0.5),
        'b_w_o': nrm((n_b, SWA_HEADS * HEAD_DIM, D), (SWA_HEADS * HEAD_DIM) ** -0.5),
        'b_sink': nrm((n_b, SWA_HEADS), 0.5),
        'c_w_qkv': nrm((n_c, D, c_cols), D ** -0.5),
        'c_w_o': nrm((n_c, GA_HEADS * HEAD_DIM, D), (GA_HEADS * HEAD_DIM) ** -0.5),
        'c_qk_norm_g': 1.0 + nrm((n_c, 2, HEAD_DIM), 0.02),
        'd_w_qkv': nrm((n_d, D, d_cols), D ** -0.5),
        'd_w_o': nrm((n_d, NA_HEADS * HEAD_DIM, D), (NA_HEADS * HEAD_DIM) ** -0.5),
        'd_rpb': nrm((n_d, NA_HEADS, 2 * NA_ROWS - 1, 2 * NA_COLS - 1), 0.1),
        'moe_router_g': nrm((DEPTH, D, N_GROUPS), D ** -0.5),
        'moe_router_g_b': nrm((DEPTH, N_GROUPS), 0.01),
        'moe_router_e': nrm((DEPTH, D, N_EXPERTS), D ** -0.5),
        'moe_router_e_b': nrm((DEPTH, N_EXPERTS), 0.01),
        'moe_w_gate': nrm((DEPTH, N_EXPERTS, D, D_EXPERT), D ** -0.5),
        'moe_w_up': nrm((DEPTH, N_EXPERTS, D, D_EXPERT), D ** -0.5),
        'moe_w_down': nrm((DEPTH, N_EXPERTS, D_EXPERT, D), D_EXPERT ** -0.5),
        'final_g': 1.0 + nrm((D,), 0.02),
    }


def reference(x, c, ctx, c_ctx, mod_w, mod_b, norm1_g, norm2_g,
              a_w_qkv, a_w_o, a_lam, a_subln_g,
              b_w_qkv, b_w_o, b_sink,
              c_w_qkv, c_w_o, c_qk_norm_g,
              d_w_qkv, d_w_o, d_rpb,
              moe_router_g, moe_router_g_b, moe_router_e, moe_router_e_b,
              moe_w_gate, moe_w_up, moe_w_down, final_g):
    b, s, d = x.shape
    n_ctx = ctx.shape[1]
    cos, sin = _axial_rope_tables(s)
    xc = ctx
    for i in range(DEPTH):
        m, j = i % N_MIXERS, i // N_MIXERS
        need_ctx = i < DEPTH - 1
        mod = jax.nn.silu(c) @ mod_w[i] + mod_b[i]
        sh1, sc1, g1, sh2, sc2, g2 = jnp.split(mod[:, None, :], 6, axis=-1)
        mod_c = jax.nn.silu(c_ctx) @ mod_w[i] + mod_b[i]
        shc1, scc1, gc1, shc2, scc2, gc2 = jnp.split(mod_c, 6)
        h = _modulate(_rmsnorm(x, norm1_g[i]), sh1, sc1)
        hc = _modulate(_rmsnorm(xc, norm1_g[i]), shc1, scc1)
        if m == 0:
            lam_init = 0.8 - 0.6 * math.exp(-0.3 * i)
            y, yc = _diff_mixer(h, hc, a_w_qkv[j], a_w_o[j], a_lam[j], a_subln_g[j], lam_init, cos, sin, need_ctx)
        elif m == 1:
            y, yc = _swa_mixer(h, hc, b_w_qkv[j], b_w_o[j], b_sink[j], cos, sin, need_ctx)
        elif m == 2:
            y, yc = _axial_gqa_mixer(h, hc, c_w_qkv[j], c_w_o[j], c_qk_norm_g[j], cos, sin, need_ctx)
        else:
            y, yc = _na_mixer(h, hc, d_w_qkv[j], d_w_o[j], d_rpb[j], need_ctx)
        x = x + g1 * y
        h2 = _modulate(_rmsnorm(x, norm2_g[i]), sh2, sc2).reshape(b * s, d)
        if need_ctx:
            xc = xc + gc1 * yc
            h2c = _modulate(_rmsnorm(xc, norm2_g[i]), shc2, scc2).reshape(b * n_ctx, d)
            tokens = jnp.concatenate([h2, h2c], axis=0)
        else:
            tokens = h2
        f = _hier_moe(tokens, moe_router_g[i], moe_router_g_b[i], moe_router_e[i], moe_router_e_b[i],
                      moe_w_gate[i], moe_w_up[i], moe_w_down[i])
        x = x + g2 * f[:b * s].reshape(b, s, d)
        if need_ctx:
            xc = xc + gc2 * f[b * s:].reshape(b, n_ctx, d)
    return _rmsnorm(x, final_g)
```

```python
import math
import numpy as np
import concourse.bass as bass
import concourse.mybir as mybir
from concourse.bass_utils import run_bass_kernel_spmd

F32 = mybir.dt.float32
BF16 = mybir.dt.bfloat16
AF = mybir.ActivationFunctionType
ALU = mybir.AluOpType
AX = mybir.AxisListType

D = 1024
HD = 64
GRID_W = 64
CTX = 256
NEXP = 32
DEXP = 512
EPS = 1e-6
NEG = -30000.0


def I(name, *args, **kw):
    return (name, args, kw)


class Buf:
    __slots__ = ("t", "name", "writes", "reads", "dslot", "space")

    def __init__(self, t, name, space):
        self.t = t
        self.name = name
        self.writes = []
        self.reads = []
        self.dslot = {}
        self.space = space

    def __getitem__(self, idx):
        return self.t[idx]


class Sched:
    ENG = ("pe", "dve", "act", "pool", "sp")
    NDSEM = 40

    def __init__(self, nc):
        self.nc = nc
        self.ops = {k: [] for k in self.ENG}
        self.cnt = {k: 0 for k in self.ENG}
        self.semobj = {}
        self.semcnt = {}
        for k in self.ENG:
            self.semobj[("c", k)] = nc.alloc_semaphore("c_" + k)
        self.free_slots = {"hw": [], "sw": []}
        for kind in ("hw", "sw"):
            for i in range(self.NDSEM):
                key = ("d" + kind, i)
                self.semobj[key] = nc.alloc_semaphore("d%s_%d" % (kind, i))
                self.semcnt[key] = 0
                self.free_slots[kind].append(key)
        self.semobj[("cc", 0)] = nc.alloc_semaphore("cc_sem")
        self.semcnt[("cc", 0)] = 0
        self.waited = {k: {} for k in self.ENG}
        self.nbuf = 0
        self.n_inst = 0
        self.out_events = []
        self.phase_bufs = []
        self.marks = []

    def sb(self, shape, dtype, name=None):
        self.nbuf += 1
        name = "%s_%d" % (name or "sb", self.nbuf)
        t = self.nc.alloc_sbuf_tensor(name, list(shape), dtype)
        b = Buf(t, name, "sb")
        self.phase_bufs.append(b)
        return b

    def ps(self, shape, dtype=F32, name=None):
        self.nbuf += 1
        name = "%s_%d" % (name or "ps", self.nbuf)
        t = self.nc.alloc_psum_tensor(name, list(shape), dtype)
        b = Buf(t, name, "ps")
        self.phase_bufs.append(b)
        return b

    def dram(self, name, shape, dtype, kind="Internal"):
        t = self.nc.dram_tensor(name, list(shape), dtype, kind=kind)
        return Buf(t, name, "dram")

    def phase_begin(self):
        nc = self.nc
        self.marks.append((nc.sbuf_base, nc.sbuf_top, nc.psum_base, nc.psum_top, len(self.phase_bufs)))

    def phase_end(self):
        self.barrier()
        nc = self.nc
        sb0, sb1, p0, p1, nb = self.marks.pop()
        for b in self.phase_bufs[nb:]:
            for kind, key in b.dslot.items():
                self.free_slots[kind].append(key)
            b.dslot = {}
        del self.phase_bufs[nb:]
        nc.sbuf_base, nc.sbuf_top = sb0, sb1
        nc.psum_base, nc.psum_top = p0, p1

    def _deps(self, reads, writes):
        deps = []
        for b in reads:
            deps.extend(b.writes)
        for b in writes:
            deps.extend(b.writes)
            deps.extend(b.reads)
        return deps

    def _emit_waits(self, eng, deps, skip_self_pe=False):
        need = {}
        w = self.waited[eng]
        for (key, val) in deps:
            if skip_self_pe and key == ("c", "pe"):
                continue
            if key[0] in ("dhw", "dsw"):
                val = self.semcnt[key]
            if w.get(key, 0) >= val:
                continue
            if need.get(key, 0) < val:
                need[key] = val
        for key, val in need.items():
            self.ops[eng].append(("wait", self.semobj[key], val))
            w[key] = val

    def _commit(self, ev, reads, writes, partial):
        for b in reads:
            b.reads.append(ev)
        for b in writes:
            if partial:
                b.writes.append(ev)
            else:
                b.writes = [ev]
            b.reads = []

    def op(self, eng, fn, reads=(), writes=(), pe_acc=False, partial=False):
        self._emit_waits(eng, self._deps(reads, writes), skip_self_pe=(pe_acc and eng == "pe"))
        self.cnt[eng] += 1
        ev = (("c", eng), self.cnt[eng])
        self.ops[eng].append(("op", fn, self.semobj[("c", eng)]))
        self._commit(ev, reads, writes, partial)
        self.n_inst += 1
        return ev

    def dma(self, q, out_ap, in_ap, reads=(), writes=(), sembuf=None, partial=False, is_output=False):
        self._emit_waits(q, self._deps(reads, writes))
        if sembuf is None:
            cands = [b for b in list(writes) + list(reads) if b.space == "sb"]
            sembuf = cands[0] if cands else (list(writes) + list(reads))[0]
        kind = "sw" if q == "pool" else "hw"
        if kind not in sembuf.dslot:
            sembuf.dslot[kind] = self.free_slots[kind].pop(0)
            if sembuf.space == "dram" and sembuf not in self.phase_bufs:
                self.phase_bufs.append(sembuf)
        key = sembuf.dslot[kind]
        self.semcnt[key] += 16
        ev = (key, self.semcnt[key])
        self.ops[q].append(("dma", out_ap, in_ap, self.semobj[key]))
        self._commit(ev, reads, writes, partial)
        if is_output:
            self.out_events.append(ev)
        self.n_inst += 1
        return ev

    def collective(self, kind, in_ap, out_ap, groups, reads=(), writes=()):
        self._emit_waits("pool", self._deps(reads, writes))
        key = ("cc", 0)
        self.semcnt[key] += 1
        ev = (key, self.semcnt[key])
        self.ops["pool"].append(("cc", kind, in_ap, out_ap, groups, self.semobj[key]))
        self._commit(ev, reads, writes, False)
        return ev

    def barrier(self):
        evs = [(("c", k), self.cnt[k]) for k in self.ENG]
        for key, v in self.semcnt.items():
            if v > 0:
                evs.append((key, v))
        for k in self.ENG:
            self._emit_waits(k, [e for e in evs if e[0] != ("c", k)])

    def finish(self):
        self._emit_waits("sp", self.out_events)
        self.barrier()
        nc = self.nc
        allsems = list(self.semobj.values())
        with nc.Block() as block0:
            def clr(engine):
                for s_ in allsems:
                    engine.sem_clear(s_)
            block0.gpsimd(clr)
        with nc.Block() as block:
            for k, deco in (("pe", block.tensor), ("dve", block.vector), ("act", block.scalar),
                            ("pool", block.gpsimd), ("sp", block.sync)):
                ops = self.ops[k]

                def body(engine, ops=ops):
                    for o in ops:
                        if o[0] == "wait":
                            engine.wait_ge(o[1], o[2])
                        elif o[0] == "op":
                            nm, a_, kw_ = o[1]
                            getattr(engine, nm)(*a_, **kw_).then_inc(o[2], 1)
                        elif o[0] == "dma":
                            engine.dma_start(out=o[1], in_=o[2]).then_inc(o[3], 16)
                        else:
                            engine.collective_compute(o[1], ALU.bypass, replica_groups=o[4],
                                                      ins=[o[2]], outs=[o[3]]).then_inc(o[5], 1)
                deco(body)
        with nc.Block() as block2:
            def clr2(engine):
                for s_ in allsems:
                    engine.sem_clear(s_)
            block2.gpsimd(clr2)
        return nc


class Cfg:
    def __init__(self, ncore=8, TL=16, depth=4):
        self.ncore = ncore
        self.TL = TL
        self.TC = CTX // 128
        self.T = TL + self.TC
        self.SEQ = ncore * TL * 128
        self.rows = self.SEQ // GRID_W
        self.depth = depth


MIX = {0: (3072, 16, 8, 128), 1: (1536, 4, 4, 64), 2: (1536, 4, 4, 64), 3: (3072, 16, 16, 64)}


def build(cfg):
    nc = bass.Bass("TRN2", target_bir_lowering=False)
    S = Sched(nc)
    ncore, TL, TC, T = cfg.ncore, cfg.TL, cfg.TC, cfg.T
    NTOK = T * 128
    groups = [list(range(ncore))]

    def ext(name, shape, dt=F32):
        return S.dram(name, shape, dt, kind="ExternalInput")

    xin = ext("xin", [T, 128, D])
    cc = ext("cc", [2, D])
    mod_w = ext("mod_w", [4, D, 6 * D])
    mod_b = ext("mod_b", [4, 6 * D])
    n1g = ext("n1g", [4, D])
    n2g = ext("n2g", [4, D])
    fing = ext("fing", [1, D])
    wqkv = [ext("wqkv%d" % m, [D, MIX[m][0]]) for m in range(4)]
    wo = [ext("wo%d" % m, [D, D]) for m in range(4)]
    a_lam = ext("a_lam", [4, HD])
    a_sub = ext("a_sub", [1, 128])
    b_sink = ext("b_sink", [1, 16])
    c_qkg = ext("c_qkg", [2, HD])
    bias3 = ext("bias3", [5, 16, 128, 5, 128])
    mask1 = ext("mask1", [128, 3, 128])
    cosT = ext("cosT", [T, 128, HD])
    sinT = ext("sinT", [T, 128, HD])
    selT = ext("selT", [128, 2 * ncore])
    wr = ext("wr", [4, D, 36])
    br = ext("br", [4, 36])
    wg = ext("moe_wg", [4, NEXP, D, DEXP])
    wu = ext("moe_wu", [4, NEXP, D, DEXP])
    wd = ext("moe_wd", [4, NEXP, DEXP, D])
    out = S.dram("out", [TL, 128, D], F32, kind="ExternalOutput")

    xbuf = S.dram("xbuf", [T, 128, D], F32)
    modr = S.dram("modr", [2, 6 * D], F32)
    qt_d = S.dram("qt_d", [16, HD, NTOK], BF16)
    NSRCS = {m_: MIX[m_][1] * HD * TL * 128 + MIX[m_][2] * 128 * TL * (MIX[m_][3] + 1) for m_ in range(4)}
    kvsrcs = {m_: S.dram("kvsrc%d" % m_, [1, NSRCS[m_]], BF16) for m_ in range(4)}
    kvgs = {m_: S.dram("kvg%d" % m_, [ncore, NSRCS[m_]], BF16) for m_ in (0, 2)}
    ao_d = S.dram("ao_d", [T, 128, D], BF16)
    kc_d = S.dram("kc_d", [16, HD, CTX], BF16)
    vc_d = S.dram("vc_d", [16 * 128 * TC * 129], BF16)
    HWS = {1: 1, 3: 2}
    HKS = {m_: MIX[m_][1] * HD * HWS[m_] * 128 for m_ in (1, 3)}
    HVS = {m_: MIX[m_][2] * 128 * HWS[m_] * (MIX[m_][3] + 1) for m_ in (1, 3)}
    hsrcs = {m_: S.dram("hsrc%d" % m_, [1, 2 * (HKS[m_] + HVS[m_])], BF16) for m_ in (1, 3)}
    hgs = {m_: S.dram("hg%d" % m_, [ncore, 2 * (HKS[m_] + HVS[m_])], BF16) for m_ in (1, 3)}
    kext = S.dram("kext", [16 * HD * (TL + 4) * 128], BF16)
    vext = S.dram("vext", [16 * 128 * (TL + 4) * 65], BF16)

    def dview(buf, off, pattern):
        return bass.AP(buf.t, off, pattern)

    identf = S.sb([128, 128], F32, "identf")
    identb = S.sb([128, 128], BF16, "identb")
    S.op("pool", I("memset", identf[:], 0.0), writes=[identf])
    S.op("pool", I("affine_select", out=identf[:], in_=identf[:], pattern=[[-1, 128]],
                                           compare_op=ALU.not_equal, fill=1.0, base=0, channel_multiplier=1),
         reads=[identf], writes=[identf])
    S.op("dve", I("tensor_copy", out=identb[:], in_=identf[:]), reads=[identf], writes=[identb])
    zer = S.sb([128, 512], BF16, "zer")
    S.op("pool", I("memset", zer[:], 0.0), writes=[zer])

    S.phase_begin()
    for t in range(T):
        xt = S.sb([128, D], F32, "xcp")
        S.dma("sp", xt[:], xin[t], reads=[xin], writes=[xt])
        S.dma("sp", xbuf[t], xt[:], reads=[xt], writes=[xbuf], partial=True)
    S.phase_end()

    def bc_load(q, dst, src_buf, row_ap):
        S.dma(q, dst[:], row_ap.partition_broadcast(128), reads=[src_buf], writes=[dst])

    def rms_rstd(xt_ap, xt_buf, rstd, junk, n):
        ssb = S.sb([128, 1], F32, "ss")
        S.op("act", I("activation", out=junk[:], in_=xt_ap, func=AF.Square, accum_out=ssb[:]),
             reads=[xt_buf], writes=[junk, ssb])
        S.op("dve", I("tensor_scalar", out=rstd[:], in0=ssb[:], scalar1=1.0 / n, scalar2=EPS,
                                              op0=ALU.mult, op1=ALU.add), reads=[ssb], writes=[rstd])
        S.op("act", I("activation", out=rstd[:], in_=rstd[:], func=AF.Sqrt), reads=[rstd], writes=[rstd])
        S.op("dve", I("reciprocal", out=rstd[:], in_=rstd[:]), reads=[rstd], writes=[rstd])

    for li in range(cfg.depth):
        mixer = li % 4
        ncols, nk, nv, dv = MIX[mixer]
        dv1 = dv + 1
        need_ctx = li < cfg.depth - 1
        local = mixer in (1, 3)
        hw = 1 if mixer == 1 else 2
        KOFF = 0
        VOFF = nk * HD * TL * 128
        lam_init = 0.8 - 0.6 * math.exp(-0.3 * li)
        kvsrc = kvsrcs[mixer]
        kvg = kvgs.get(mixer)
        NSRC = NSRCS[mixer]
        if local:
            HK, HV = HKS[mixer], HVS[mixer]
            HS = 2 * (HK + HV)
            hsrc, hg = hsrcs[mixer], hgs[mixer]

        S.phase_begin()
        cct = S.sb([2, D], F32, "cct")
        S.dma("sp", cct[:], cc[:], reads=[cc], writes=[cct])
        S.op("act", I("activation", out=cct[:], in_=cct[:], func=AF.Silu), reads=[cct], writes=[cct])
        psT = S.ps([128, 8, 2], F32, "psT")
        for c in range(8):
            S.op("pe", I("transpose", out=psT[:, c, :], in_=cct[:, c * 128:(c + 1) * 128],
                                                  identity=identf[0:2, 0:2]),
                 reads=[cct, identf], writes=[psT], partial=True, pe_acc=True)
        sT = S.sb([128, 8, 2], F32, "sT")
        S.op("dve", I("tensor_copy", out=sT[:], in_=psT[:]), reads=[psT], writes=[sT])
        mrow = S.sb([2, 6 * D], F32, "mrow")
        mbt = S.sb([2, 6 * D], F32, "mbt")
        S.dma("sp", mbt[:], dview(mod_b, li * 6 * D, [[0, 2], [1, 6 * D]]), reads=[mod_b], writes=[mbt])
        mwb = [S.sb([128, 8, 512], F32, "mw") for _ in range(2)]
        pm = [S.ps([2, 512], F32, "pm") for _ in range(2)]
        for j in range(12):
            w_ = mwb[j % 2]
            S.dma("sp" if j % 2 == 0 else "act", w_[:],
                  mod_w[li, :, j * 512:(j + 1) * 512].rearrange("(c p) n -> p c n", p=128),
                  reads=[mod_w], writes=[w_])
            p_ = pm[j % 2]
            for c in range(8):
                S.op("pe", I("matmul", p_[:], lhsT=sT[:, c, :], rhs=w_[:, c, :],
                                                                 start=(c == 0), stop=(c == 7)),
                     reads=[sT, w_], writes=[p_], pe_acc=(c > 0))
            S.op("dve", I("tensor_tensor", out=mrow[:, j * 512:(j + 1) * 512], in0=p_[:],
                                                              in1=mbt[:, j * 512:(j + 1) * 512], op=ALU.add),
                 reads=[p_, mbt], writes=[mrow], partial=True)
        S.dma("sp", modr[:], mrow[:], reads=[mrow], writes=[modr])
        S.phase_end()

        def load_mod(which, vec, dst, q="sp"):
            bc_load(q, dst, modr, modr[which:which + 1, vec * D:(vec + 1) * D])

        S.phase_begin()
        gbc = S.sb([128, D], F32, "gbc")
        bc_load("sp", gbc, n1g, n1g[li:li + 1, :])
        A1 = [S.sb([128, D], F32, "A1") for _ in range(2)]
        B1 = [S.sb([128, D], F32, "B1") for _ in range(2)]
        for w_ in range(2):
            load_mod(w_, 1, A1[w_])
            S.op("dve", I("scalar_tensor_tensor", out=A1[w_][:], in0=A1[w_][:], scalar=1.0, in1=gbc[:],
                                                                op0=ALU.add, op1=ALU.mult),
                 reads=[A1[w_], gbc], writes=[A1[w_]])
            load_mod(w_, 0, B1[w_])
        wq = S.sb([128, 8, ncols], BF16, "wq")
        wst = [S.sb([128, 8, 512], F32, "wst") for _ in range(2)]
        for j in range(ncols // 512):
            s_ = wst[j % 2]
            S.dma("sp" if j % 2 == 0 else "act", s_[:],
                  wqkv[mixer][:, j * 512:(j + 1) * 512].rearrange("(c p) n -> p c n", p=128),
                  reads=[wqkv[mixer]], writes=[s_])
            S.op("pool", I("tensor_copy", out=wq[:, :, j * 512:(j + 1) * 512], in_=s_[:]),
                 reads=[s_], writes=[wq], partial=True)
        if mixer == 2:
            qg = S.sb([128, HD], F32, "qg")
            kg = S.sb([128, HD], F32, "kg")
            bc_load("sp", qg, c_qkg, c_qkg[0:1, :])
            bc_load("sp", kg, c_qkg, c_qkg[1:2, :])
        NB = 2
        xts = [S.sb([128, D], F32, "xt") for _ in range(NB)]
        junk = S.sb([128, D], F32, "junk")
        tmpf = S.sb([128, D], F32, "tmpf")
        hbs = [S.sb([128, D], BF16, "hb") for _ in range(NB)]
        hTs = [S.sb([128, 8, 128], BF16, "hT") for _ in range(NB)]
        pTs = [S.ps([128, 8, 128], BF16, "pT") for _ in range(NB)]
        pq = [S.ps([128, 512], F32, "pq") for _ in range(2)]
        qkvs = [S.sb([128, ncols], F32, "qkv") for _ in range(NB)]
        cosb = [S.sb([128, HD], F32, "cos") for _ in range(NB)]
        sinb = [S.sb([128, HD], F32, "sin") for _ in range(NB)]
        ra = S.sb([128, 16 * 32], F32, "ra")
        rb = S.sb([128, 16 * 32], F32, "rb")
        sq = S.sb([128, 16 * HD], F32, "sq")
        qb = [S.sb([128, 16, HD], BF16, "qb") for _ in range(NB)]
        kb = [S.sb([128, nk, HD], BF16, "kb") for _ in range(NB)]
        va = [S.sb([128, nv, dv1], BF16, "va") for _ in range(NB)]
        for v_ in va:
            S.op("pool", I("memset", v_[:], 1.0), writes=[v_])
        pqT = [S.ps([HD, 16, 128], BF16, "pqT") for _ in range(1)]
        qTs = [S.sb([HD, 16, 128], BF16, "qTs") for _ in range(NB)]
        kTs = [S.sb([HD, nk, 128], BF16, "kTs") for _ in range(NB)]
        rstds = [S.sb([128, 1], F32, "rstd") for _ in range(NB)]
        nrm = S.sb([128, 16], F32, "nrm")

        def rope(src_buf, src_ap, dst_buf, dst_ap, nh, cb, sb_):
            sv = src_ap.rearrange("p (h a b d) -> p h a b d", a=2, b=2, d=16)
            dvw = dst_ap.rearrange("p h (a b d) -> p h a b d", a=2, b=2, d=16)
            cv = cb[:].rearrange("p (a b d) -> p a b d", a=2, b=2)[:, :, 0, :].unsqueeze(1).broadcast_to([128, nh, 2, 16])
            sn = sb_[:].rearrange("p (a b d) -> p a b d", a=2, b=2)[:, :, 0, :].unsqueeze(1).broadcast_to([128, nh, 2, 16])
            x1 = sv[:, :, :, 0, :]
            x2 = sv[:, :, :, 1, :]
            rav = ra[:, 0:nh * 32].rearrange("p (h a d) -> p h a d", a=2, d=16)
            rbv = rb[:, 0:nh * 32].rearrange("p (h a d) -> p h a d", a=2, d=16)
            S.op("dve", I("tensor_tensor", out=rav, in0=x1, in1=cv, op=ALU.mult), reads=[src_buf, cb], writes=[ra])
            S.op("dve", I("tensor_tensor", out=rbv, in0=x2, in1=sn, op=ALU.mult), reads=[src_buf, sb_], writes=[rb])
            S.op("dve", I("tensor_tensor", out=dvw[:, :, :, 0, :], in0=rav, in1=rbv, op=ALU.subtract),
                 reads=[ra, rb], writes=[dst_buf], partial=True)
            S.op("dve", I("tensor_tensor", out=rav, in0=x2, in1=cv, op=ALU.mult), reads=[src_buf, cb], writes=[ra])
            S.op("dve", I("tensor_tensor", out=rbv, in0=x1, in1=sn, op=ALU.mult), reads=[src_buf, sb_], writes=[rb])
            S.op("dve", I("tensor_tensor", out=dvw[:, :, :, 1, :], in0=rav, in1=rbv, op=ALU.add),
                 reads=[ra, rb], writes=[dst_buf], partial=True)

        def headnorm(buf, ap, nh, gtile):
            v3 = ap.rearrange("p (h d) -> p h d", d=HD)
            sq3 = sq[:, 0:nh * HD].rearrange("p (h d) -> p h d", d=HD)
            S.op("dve", I("tensor_tensor", out=sq3, in0=v3, in1=v3, op=ALU.mult), reads=[buf], writes=[sq])
            S.op("dve", I("tensor_reduce", out=nrm[:, 0:nh], in_=sq3, axis=AX.X, op=ALU.add), reads=[sq], writes=[nrm])
            S.op("dve", I("tensor_scalar", out=nrm[:, 0:nh], in0=nrm[:, 0:nh], scalar1=1.0 / HD, scalar2=EPS,
                                                  op0=ALU.mult, op1=ALU.add), reads=[nrm], writes=[nrm])
            S.op("act", I("activation", out=nrm[:, 0:nh], in_=nrm[:, 0:nh], func=AF.Sqrt), reads=[nrm], writes=[nrm])
            S.op("dve", I("reciprocal", out=nrm[:, 0:nh], in_=nrm[:, 0:nh]), reads=[nrm], writes=[nrm])
            S.op("dve", I("tensor_tensor", out=v3, in0=v3, in1=nrm[:, 0:nh].unsqueeze(2).broadcast_to([128, nh, HD]),
                                                  op=ALU.mult), reads=[buf, nrm], writes=[buf])
            S.op("dve", I("tensor_tensor", out=v3, in0=v3, in1=gtile[:].unsqueeze(1).broadcast_to([128, nh, HD]),
                                                  op=ALU.mult), reads=[buf, gtile], writes=[buf])

        for t in range(T):
            i = t % NB
            isctx = 1 if t >= TL else 0
            xt, hb, hT, pT, qkv = xts[i], hbs[i], hTs[i], pTs[i], qkvs[i]
            S.dma("sp", xt[:], xbuf[t], reads=[xbuf], writes=[xt])
            if mixer != 3:
                S.dma("act", cosb[i][:], cosT[t], reads=[cosT], writes=[cosb[i]])
                S.dma("act", sinb[i][:], sinT[t], reads=[sinT], writes=[sinb[i]])
            rms_rstd(xt[:], xt, rstds[i], junk, D)
            S.op("dve", I("scalar_tensor_tensor",
                out=tmpf[:], in0=xt[:], scalar=rstds[i][:], in1=A1[isctx][:], op0=ALU.mult, op1=ALU.mult),
                reads=[xt, rstds[i], A1[isctx]], writes=[tmpf])
            S.op("dve", I("tensor_tensor", out=hb[:], in0=tmpf[:], in1=B1[isctx][:], op=ALU.add),
                 reads=[tmpf, B1[isctx]], writes=[hb])
            for c in range(8):
                S.op("pe", I("transpose", out=pT[:, c, :], in_=hb[:, c * 128:(c + 1) * 128],
                                                                   identity=identb[:]),
                     reads=[hb, identb], writes=[pT], partial=True, pe_acc=True)
            S.op("act", I("copy", out=hT[:], in_=pT[:]), reads=[pT], writes=[hT])
            for j in range(ncols // 512):
                p_ = pq[j % 2]
                for c in range(8):
                    S.op("pe", I("matmul", p_[:], lhsT=hT[:, c, :],
                                                                         rhs=wq[:, c, j * 512:(j + 1) * 512],
                                                                         start=(c == 0), stop=(c == 7)),
                         reads=[hT, wq], writes=[p_], pe_acc=(c > 0))
                S.op("act", I("copy", out=qkv[:, j * 512:(j + 1) * 512], in_=p_[:]),
                     reads=[p_], writes=[qkv], partial=True)
            qap = qkv[:, 0:1024]
            kap = qkv[:, 1024:1024 + nk * HD]
            vap = qkv[:, 1024 + nk * HD:ncols]
            if mixer == 2:
                headnorm(qkv, qap, 16, qg)
                headnorm(qkv, kap, nk, kg)
            if mixer != 3:
                rope(qkv, qap, qb[i], qb[i][:], 16, cosb[i], sinb[i])
                rope(qkv, kap, kb[i], kb[i][:], nk, cosb[i], sinb[i])
            else:
                S.op("dve", I("tensor_copy", out=qb[i][:], in_=qap.rearrange("p (h d) -> p h d", d=HD)),
                     reads=[qkv], writes=[qb[i]])
                S.op("dve", I("tensor_copy", out=kb[i][:], in_=kap.rearrange("p (h d) -> p h d", d=HD)),
                     reads=[qkv], writes=[kb[i]])
            S.op("pool", I("tensor_copy", out=va[i][:, :, 0:dv], in_=vap.rearrange("p (h d) -> p h d", d=dv)),
                 reads=[qkv], writes=[va[i]])
            pqt = pqT[0]
            for h in range(16):
                S.op("pe", I("transpose", out=pqt[:, h, :], in_=qb[i][:, h, :], identity=identb[:]),
                     reads=[qb[i], identb], writes=[pqt], partial=True, pe_acc=True)
            S.op("act", I("copy", out=qTs[i][:], in_=pqt[:]), reads=[pqt], writes=[qTs[i]])
            S.dma("pool", qt_d[:, :, t * 128:(t + 1) * 128].rearrange("h d n -> d h n"), qTs[i][:],
                  reads=[qTs[i]], writes=[qt_d], partial=True)
            for h in range(nk):
                S.op("pe", I("transpose", out=pqt[:, h, :], in_=kb[i][:, h, :], identity=identb[:]),
                     reads=[kb[i], identb], writes=[pqt], partial=True, pe_acc=True)
            S.op("act", I("copy", out=kTs[i][:], in_=pqt[:, 0:nk, :]), reads=[pqt], writes=[kTs[i]])
            if not isctx:
                kdst = dview(kvsrc, KOFF + t * 128, [[TL * 128, HD], [HD * TL * 128, nk], [1, 128]])
                vdst = dview(kvsrc, VOFF + t * dv1, [[TL * dv1, 128], [128 * TL * dv1, nv], [1, dv1]])
                S.dma("pool", kdst, kTs[i][:], reads=[kTs[i]], writes=[kvsrc], partial=True)
                S.dma("pool", vdst, va[i][:], reads=[va[i]], writes=[kvsrc], partial=True)
                if local and (t < hw or t >= TL - hw):
                    which = 0 if t < hw else 1
                    tt = t if t < hw else t - (TL - hw)
                    hoff = which * (HK + HV)
                    kd2 = dview(hsrc, hoff + tt * 128, [[hw * 128, HD], [HD * hw * 128, nk], [1, 128]])
                    vd2 = dview(hsrc, hoff + HK + tt * dv1, [[hw * dv1, 128], [128 * hw * dv1, nv], [1, dv1]])
                    S.dma("pool", kd2, kTs[i][:], reads=[kTs[i]], writes=[hsrc], partial=True)
                    S.dma("pool", vd2, va[i][:], reads=[va[i]], writes=[hsrc], partial=True)
            else:
                tt = t - TL
                kdst = dview(kc_d, tt * 128, [[CTX, HD], [HD * CTX, nk], [1, 128]])
                vdst = dview(vc_d, tt * dv1, [[TC * dv1, 128], [128 * TC * dv1, nv], [1, dv1]])
                S.dma("pool", kdst, kTs[i][:], reads=[kTs[i]], writes=[kc_d], partial=True)
                S.dma("pool", vdst, va[i][:], reads=[va[i]], writes=[vc_d], partial=True)
        S.phase_end()

        S.phase_begin()
        if not local:
            S.collective("AllGather", kvsrc.t.ap().opt(), kvg.t.ap().opt(), groups, reads=[kvsrc], writes=[kvg])
            NL = ncore * TL
        else:
            NL = TL + 2 * hw
            S.collective("AllGather", hsrc.t.ap().opt(), hg.t.ap().opt(), groups, reads=[hsrc], writes=[hg])
            selb = S.sb([128, 2 * ncore], F32, "selb")
            S.dma("sp", selb[:], selT[:], reads=[selT], writes=[selb])
            S.dma("sp", dview(kext, hw * 128, [[NL * 128, nk * HD], [1, TL * 128]]),
                  dview(kvsrc, KOFF, [[TL * 128, nk * HD], [1, TL * 128]]), reads=[kvsrc], writes=[kext], partial=True)
            S.dma("act", dview(vext, hw * dv1, [[NL * dv1, nv * 128], [1, TL * dv1]]),
                  dview(kvsrc, VOFF, [[TL * dv1, nv * 128], [1, TL * dv1]]), reads=[kvsrc], writes=[vext], partial=True)
            for side in range(2):
                which = 1 if side == 0 else 0
                hoff = which * (HK + HV)
                kacc = S.sb([HD, nk, hw * 128], BF16, "kacc")
                vacc = S.sb([128, nv, hw * dv1], BF16, "vacc")
                S.op("pool", I("memset", kacc[:], 0.0), writes=[kacc])
                S.op("pool", I("memset", vacc[:], 0.0), writes=[vacc])
                kin = [S.sb([HD, nk, hw * 128], BF16, "kin") for _ in range(2)]
                vin = [S.sb([128, nv, hw * dv1], BF16, "vin") for _ in range(2)]
                for r in range(ncore):
                    k_, v_ = kin[r % 2], vin[r % 2]
                    S.dma("sp", k_[:], dview(hg, r * HS + hoff, [[hw * 128, HD], [HD * hw * 128, nk], [1, hw * 128]]),
                          reads=[hg], writes=[k_])
                    S.dma("act", v_[:], dview(hg, r * HS + hoff + HK, [[hw * dv1, 128], [128 * hw * dv1, nv], [1, hw * dv1]]),
                          reads=[hg], writes=[v_])
                    col = side * ncore + r
                    S.op("dve", I("scalar_tensor_tensor",
                        out=kacc[:], in0=k_[:], scalar=selb[0:HD, col:col + 1], in1=kacc[:], op0=ALU.mult, op1=ALU.add),
                        reads=[k_, selb, kacc], writes=[kacc])
                    S.op("dve", I("scalar_tensor_tensor",
                        out=vacc[:], in0=v_[:], scalar=selb[:, col:col + 1], in1=vacc[:], op0=ALU.mult, op1=ALU.add),
                        reads=[v_, selb, vacc], writes=[vacc])
                toff = 0 if side == 0 else hw + TL
                S.dma("sp", dview(kext, toff * 128, [[NL * 128, HD], [HD * NL * 128, nk], [1, hw * 128]]), kacc[:],
                      reads=[kacc], writes=[kext], partial=True)
                S.dma("sp", dview(vext, toff * dv1, [[NL * dv1, 128], [128 * NL * dv1, nv], [1, hw * dv1]]), vacc[:],
                      reads=[vacc], writes=[vext], partial=True)
        S.phase_end()

        S.phase_begin()
        NKT = TC + NL
        aost = [S.sb([128, 128], BF16, "aost") for _ in range(4)]
        aoc = [0]
        chc = [0]
        ngroup = 8 if mixer == 0 else 16
        gsz = 2 if mixer == 0 else 1
        KT = [S.sb([HD, NKT * 128], BF16, "KT") for _ in range(2)]
        VT = [S.sb([128, NKT, dv1], BF16, "VT") for _ in range(2)]
        QT = [S.sb([HD, NTOK], BF16, "QT") for _ in range(2 * gsz)]
        pS = [S.ps([128, 512], F32, "pS") for _ in range(2)]
        nacc = 2 if dv1 > 128 else 1
        nset = 1 if mixer == 0 else 2
        pA = [[S.ps([128, 4 // nacc, dv1], F32, "pA") for _ in range(nacc)] for _ in range(nset * gsz)]
        PTs = [S.sb([128, 512], BF16, "PT") for _ in range(3)]
        sbias = [S.sb([128, 512], F32, "sbias") for _ in range(2)]
        if mixer == 1:
            m1 = S.sb([128, 3, 128], F32, "m1")
            S.dma("sp", m1[:], mask1[:], reads=[mask1], writes=[m1])
            sk = S.sb([128, 16], F32, "sk")
            bc_load("sp", sk, b_sink, b_sink[0:1, :])
            S.op("act", I("activation", out=sk[:], in_=sk[:], func=AF.Exp), reads=[sk], writes=[sk])
        if mixer == 3:
            b3 = [S.sb([128, 5, 128], F32, "b3") for _ in range(2)]
        if mixer == 0:
            lamt = S.sb([128, 4, HD], F32, "lamt")
            S.dma("sp", lamt[:], dview(a_lam, 0, [[0, 128], [1, 4 * HD]]), reads=[a_lam], writes=[lamt])
            lp = S.sb([128, 2, HD], F32, "lp")
            S.op("dve", I("tensor_tensor", out=lp[:, 0, :], in0=lamt[:, 0, :], in1=lamt[:, 1, :], op=ALU.mult),
                 reads=[lamt], writes=[lp], partial=True)
            S.op("dve", I("tensor_tensor", out=lp[:, 1, :], in0=lamt[:, 2, :], in1=lamt[:, 3, :], op=ALU.mult),
                 reads=[lamt], writes=[lp], partial=True)
            ls = S.sb([128, 2], F32, "ls")
            S.op("dve", I("tensor_reduce", out=ls[:], in_=lp[:], axis=AX.X, op=ALU.add), reads=[lp], writes=[ls])
            S.op("act", I("activation", out=ls[:], in_=ls[:], func=AF.Exp), reads=[ls], writes=[ls])
            nlam = S.sb([128, 1], F32, "nlam")
            S.op("dve", I("tensor_tensor", out=nlam[:], in0=ls[:, 1:2], in1=ls[:, 0:1], op=ALU.subtract),
                 reads=[ls], writes=[nlam])
            S.op("dve", I("tensor_scalar", out=nlam[:], in0=nlam[:], scalar1=-lam_init, scalar2=None, op0=ALU.add),
                 reads=[nlam], writes=[nlam])
            subg = S.sb([128, 128], F32, "subg")
            bc_load("sp", subg, a_sub, a_sub[0:1, :])
            S.op("dve", I("tensor_scalar", out=subg[:], in0=subg[:], scalar1=1.0 - lam_init, scalar2=None, op0=ALU.mult),
                 reads=[subg], writes=[subg])
        o0 = S.sb([128, 129], F32, "o0")
        o1 = S.sb([128, 129], F32, "o1")
        rs_ = S.sb([128, 2], F32, "rs")
        jk = S.sb([128, 128], F32, "jk")
        ss1 = S.sb([128, 1], F32, "ss1")
        ptc = [0]
        psc = [0]

        def kidx_of(m):
            return m if mixer in (0, 3) else m // 4

        def vidx_of(g):
            return g if mixer in (0, 3) else g // 4

        last_k = {}
        last_v = [None, None]
        kslot = [0]
        vslot = [0]

        def load_k(kidx):
            if kidx in last_k:
                return last_k[kidx]
            b = KT[kslot[0] % 2]
            kslot[0] += 1
            for kk in [k_ for k_, v_ in last_k.items() if v_ is b]:
                del last_k[kk]
            S.dma("sp", b[:, 0:CTX], kc_d[kidx], reads=[kc_d], writes=[b])
            if local:
                S.dma("sp", b[:, CTX:NKT * 128], dview(kext, kidx * HD * NL * 128, [[NL * 128, HD], [1, NL * 128]]),
                      reads=[kext], writes=[b], partial=True)
            else:
                src = dview(kvg, KOFF + kidx * HD * TL * 128, [[TL * 128, HD], [NSRC, ncore], [1, TL * 128]])
                S.dma("sp", b[:, CTX:NKT * 128].rearrange("d (r n) -> d r n", r=ncore), src, reads=[kvg], writes=[b], partial=True)
            last_k[kidx] = b
            return b

        def load_v(vidx):
            if last_v[0] == vidx:
                return last_v[1]
            b = VT[vslot[0] % 2]
            vslot[0] += 1
            S.dma("act", b[:, 0:TC, :], dview(vc_d, vidx * 128 * TC * dv1, [[TC * dv1, 128], [dv1, TC], [1, dv1]]),
                  reads=[vc_d], writes=[b])
            if local:
                S.dma("act", b[:, TC:NKT, :], dview(vext, vidx * 128 * NL * dv1, [[NL * dv1, 128], [dv1, NL], [1, dv1]]),
                      reads=[vext], writes=[b], partial=True)
            else:
                src = dview(kvg, VOFF + vidx * 128 * TL * dv1, [[TL * dv1, 128], [NSRC, ncore], [1, TL * dv1]])
                S.dma("act", b[:, TC:NKT, :].rearrange("p (r t) e -> p r (t e)", r=ncore), src, reads=[kvg], writes=[b], partial=True)
            last_v[0], last_v[1] = vidx, b
            return b

        for g in range(ngroup):
            maps = [g * gsz + a for a in range(gsz)]
            Vb = load_v(vidx_of(g))
            Kbs = [load_k(kidx_of(m)) for m in maps]
            Qbs = []
            for a, m in enumerate(maps):
                qb_ = QT[(g % 2) * gsz + a]
                S.dma("pool", qb_[:], qt_d[m], reads=[qt_d], writes=[qb_])
                Qbs.append(qb_)
            chunks = []
            if local:
                for j in range(TL):
                    keys = [(kt, None) for kt in range(TC)]
                    for d in range(2 * hw + 1):
                        keys.append((TC + j + d, d))
                    chunks.append((j, 1, keys))
            else:
                for j in range(0, TL, 4):
                    chunks.append((j, min(4, TL - j), [(kt, None) for kt in range(NKT)]))
            if need_ctx:
                chunks.append((TL, TC, [(kt, None) for kt in range(TC)]))
            for (t0, ntl, keys) in chunks:
                nq = ntl * 128
                if mixer == 3 and t0 < TL:
                    slot = t0 if t0 < 2 else (2 + t0 - (TL - 2) if t0 >= TL - 2 else 4)
                    bb = b3[t0 % 2]
                    S.dma("sp", bb[:], bias3[slot, g], reads=[bias3], writes=[bb])
                chc[0] += 1
                aset = chc[0] % nset
                for a, m in enumerate(maps):
                    accs = pA[aset * gsz + a]
                    kb_, qb__ = Kbs[a], Qbs[a]
                    for ab in accs:
                        ncol = (4 // nacc) * dv1
                        S.op("pe", I("matmul", ab[:].rearrange("p a e -> p (a e)"),
                                                                      lhsT=zer[:, 0:128], rhs=zer[:, 0:ncol],
                                                                      start=True, stop=True, skip_group_check=True),
                             reads=[zer], writes=[ab])
                    for ki, (kt, bd) in enumerate(keys):
                        lastk = ki == len(keys) - 1
                        ps_ = pS[psc[0] % 2]
                        psc[0] += 1
                        S.op("pe", I("matmul",
                            ps_[:, 0:nq], lhsT=kb_[:, kt * 128:(kt + 1) * 128], rhs=qb__[:, t0 * 128:t0 * 128 + nq],
                            start=True, stop=True), reads=[kb_, qb__], writes=[ps_])
                        pt_ = PTs[ptc[0] % 3]
                        ptc[0] += 1
                        if bd is not None:
                            sbb = sbias[ptc[0] % 2]
                            bias_ap = m1[:, bd, :] if mixer == 1 else bb[:, bd, :]
                            bias_buf = m1 if mixer == 1 else bb
                            S.op("dve", I("scalar_tensor_tensor",
                                out=sbb[:, 0:nq], in0=ps_[:, 0:nq], scalar=0.125, in1=bias_ap, op0=ALU.mult, op1=ALU.add),
                                reads=[ps_, bias_buf], writes=[sbb])
                            S.op("act", I("activation", out=pt_[:, 0:nq], in_=sbb[:, 0:nq], func=AF.Exp),
                                 reads=[sbb], writes=[pt_])
                        else:
                            S.op("act", I("activation", out=pt_[:, 0:nq], in_=ps_[:, 0:nq],
                                                                                        func=AF.Exp, scale=0.125),
                                 reads=[ps_], writes=[pt_])
                        for jq in range(ntl):
                            ab = accs[jq // (4 // nacc)]
                            S.op("pe", I("matmul",
                                ab[:, jq % (4 // nacc), :], lhsT=pt_[:, jq * 128:(jq + 1) * 128], rhs=Vb[:, kt, :],
                                start=False, stop=True, skip_group_check=True),
                                reads=[pt_, Vb], writes=[ab], pe_acc=True, partial=True)
                for jq in range(ntl):
                    t = t0 + jq
                    accv = [pA[aset * gsz + a][jq // (4 // nacc)][:, jq % (4 // nacc), :] for a in range(gsz)]
                    accb = [pA[aset * gsz + a][jq // (4 // nacc)] for a in range(gsz)]
                    if mixer == 0:
                        S.op("act", I("copy", out=o0[:, 0:dv1], in_=accv[0]), reads=[accb[0]], writes=[o0])
                        S.op("act", I("copy", out=o1[:, 0:dv1], in_=accv[1]), reads=[accb[1]], writes=[o1])
                        S.op("dve", I("reciprocal", out=rs_[:, 0:1], in_=o0[:, dv:dv1]), reads=[o0], writes=[rs_], partial=True)
                        S.op("dve", I("reciprocal", out=rs_[:, 1:2], in_=o1[:, dv:dv1]), reads=[o1, rs_], writes=[rs_], partial=True)
                        S.op("dve", I("tensor_scalar", out=rs_[:, 1:2], in0=rs_[:, 1:2], scalar1=nlam[:], scalar2=None, op0=ALU.mult),
                             reads=[rs_, nlam], writes=[rs_])
                        S.op("dve", I("tensor_scalar", out=o0[:, 0:dv], in0=o0[:, 0:dv], scalar1=rs_[:, 0:1], scalar2=None, op0=ALU.mult),
                             reads=[o0, rs_], writes=[o0])
                        S.op("dve", I("scalar_tensor_tensor", out=o0[:, 0:dv], in0=o1[:, 0:dv], scalar=rs_[:, 1:2], in1=o0[:, 0:dv],
                                                                     op0=ALU.mult, op1=ALU.add), reads=[o0, o1, rs_], writes=[o0])
                        S.op("act", I("activation", out=jk[:], in_=o0[:, 0:dv], func=AF.Square, accum_out=ss1[:]),
                             reads=[o0], writes=[jk, ss1])
                        S.op("dve", I("tensor_scalar", out=ss1[:], in0=ss1[:], scalar1=1.0 / dv, scalar2=EPS, op0=ALU.mult, op1=ALU.add),
                             reads=[ss1], writes=[ss1])
                        S.op("act", I("activation", out=ss1[:], in_=ss1[:], func=AF.Sqrt), reads=[ss1], writes=[ss1])
                        S.op("dve", I("reciprocal", out=ss1[:], in_=ss1[:]), reads=[ss1], writes=[ss1])
                        ast = aost[aoc[0] % 4]
                        aoc[0] += 1
                        S.op("dve", I("scalar_tensor_tensor", out=ast[:, 0:dv], in0=o0[:, 0:dv], scalar=ss1[:],
                                      in1=subg[:], op0=ALU.mult, op1=ALU.mult),
                             reads=[o0, ss1, subg], writes=[ast])
                        S.dma("pool", ao_d[t, :, g * dv:(g + 1) * dv], ast[:, 0:dv], reads=[ast], writes=[ao_d], partial=True)
                    else:
                        S.op("act", I("copy", out=o0[:, 0:dv1], in_=accv[0]), reads=[accb[0]], writes=[o0])
                        if mixer == 1:
                            S.op("dve", I("tensor_tensor", out=o0[:, dv:dv1], in0=o0[:, dv:dv1], in1=sk[:, g:g + 1], op=ALU.add),
                                 reads=[o0, sk], writes=[o0])
                        S.op("dve", I("reciprocal", out=rs_[:, 0:1], in_=o0[:, dv:dv1]), reads=[o0], writes=[rs_])
                        ast = aost[aoc[0] % 4]
                        aoc[0] += 1
                        S.op("dve", I("tensor_scalar", out=ast[:, 0:dv], in0=o0[:, 0:dv], scalar1=rs_[:, 0:1],
                                      scalar2=None, op0=ALU.mult), reads=[o0, rs_], writes=[ast])
                        S.dma("pool", ao_d[t, :, g * dv:(g + 1) * dv], ast[:, 0:dv], reads=[ast], writes=[ao_d], partial=True)
        S.phase_end()

        TP = T if need_ctx else TL
        S.phase_begin()
        H2T = S.sb([128, 8, TP * 128], BF16, "H2T")
        Wgt = S.sb([128, TP, NEXP], F32, "Wgt")
        S.phase_begin()
        wob = S.sb([128, 8, D], BF16, "wob")
        wst = [S.sb([128, 8, 512], F32, "wst") for _ in range(2)]
        for j in range(2):
            S.dma("sp" if j == 0 else "act", wst[j][:], wo[mixer][:, j * 512:(j + 1) * 512].rearrange("(c p) n -> p c n", p=128),
                  reads=[wo[mixer]], writes=[wst[j]])
            S.op("pool", I("tensor_copy", out=wob[:, :, j * 512:(j + 1) * 512], in_=wst[j][:]),
                 reads=[wst[j]], writes=[wob], partial=True)
        gbc = S.sb([128, D], F32, "gbc2")
        bc_load("sp", gbc, n2g, n2g[li:li + 1, :])
        G1 = [S.sb([128, D], F32, "G1") for _ in range(2)]
        A2 = [S.sb([128, D], F32, "A2") for _ in range(2)]
        B2 = [S.sb([128, D], F32, "B2") for _ in range(2)]
        for w_ in range(2):
            load_mod(w_, 2, G1[w_])
            load_mod(w_, 4, A2[w_], q="act")
            S.op("dve", I("scalar_tensor_tensor", out=A2[w_][:], in0=A2[w_][:], scalar=1.0, in1=gbc[:],
                                                                op0=ALU.add, op1=ALU.mult), reads=[A2[w_], gbc], writes=[A2[w_]])
            load_mod(w_, 3, B2[w_], q="act")
        wrt = S.sb([128, 8, 36], F32, "wrt")
        S.dma("sp", wrt[:], wr[li].rearrange("(c p) n -> p c n", p=128), reads=[wr], writes=[wrt])
        brt = S.sb([128, 36], F32, "brt")
        bc_load("sp", brt, br, br[li:li + 1, :])
        NB = 2
        aot = [S.sb([128, D], BF16, "aot") for _ in range(NB)]
        xts = [S.sb([128, D], F32, "xt4") for _ in range(NB)]
        aoT = [S.sb([128, 8, 128], BF16, "aoT") for _ in range(NB)]
        pT4 = [S.ps([128, 8, 128], BF16, "pT4") for _ in range(1)]
        py = [S.ps([128, 512], F32, "py") for _ in range(2)]
        ptf = S.ps([128, 8, 128], F32, "ptf")
        plg = S.ps([128, 36], F32, "plg")
        junk = S.sb([128, D], F32, "junk4")
        tmpf = S.sb([128, D], F32, "tmpf4")
        h2f = S.sb([128, D], F32, "h2f")
        h2b = S.sb([128, D], BF16, "h2b")
        h2T32 = S.sb([128, 8, 128], F32, "h2T32")
        rstd4 = [S.sb([128, 1], F32, "rstd4") for _ in range(NB)]
        lgt = S.sb([128, 36], F32, "lgt")
        sm = S.sb([128, 16], F32, "sm")
        ohg = S.sb([128, 4], F32, "ohg")
        lem = S.sb([128, 32], F32, "lem")
        top8 = S.sb([128, 8], F32, "top8")
        mk1 = S.sb([128, 32], F32, "mk1")
        mk2 = S.sb([128, 32], F32, "mk2")
        for t in range(TP):
            i = t % NB
            isctx = 1 if t >= TL else 0
            xt = xts[i]
            S.dma("sp", xt[:], xbuf[t], reads=[xbuf], writes=[xt])
            S.dma("act", aot[i][:], ao_d[t], reads=[ao_d], writes=[aot[i]])
            pT = pT4[0]
            for c in range(8):
                S.op("pe", I("transpose", out=pT[:, c, :], in_=aot[i][:, c * 128:(c + 1) * 128], identity=identb[:]),
                     reads=[aot[i], identb], writes=[pT], partial=True, pe_acc=True)
            S.op("act", I("copy", out=aoT[i][:], in_=pT[:]), reads=[pT], writes=[aoT[i]])
            for j in range(2):
                for c in range(8):
                    S.op("pe", I("matmul", py[j][:], lhsT=aoT[i][:, c, :], rhs=wob[:, c, j * 512:(j + 1) * 512],
                                                                start=(c == 0), stop=(c == 7)),
                         reads=[aoT[i], wob], writes=[py[j]], pe_acc=(c > 0))
                S.op("dve", I("tensor_tensor", out=tmpf[:, j * 512:(j + 1) * 512], in0=py[j][:],
                                                                        in1=G1[isctx][:, j * 512:(j + 1) * 512], op=ALU.mult),
                     reads=[py[j], G1[isctx]], writes=[tmpf], partial=True)
            S.op("dve", I("tensor_tensor", out=xt[:], in0=xt[:], in1=tmpf[:], op=ALU.add), reads=[xt, tmpf], writes=[xt])
            S.dma("pool", xbuf[t], xt[:], reads=[xt], writes=[xbuf], partial=True)
            rms_rstd(xt[:], xt, rstd4[i], junk, D)
            S.op("dve", I("scalar_tensor_tensor", out=tmpf[:], in0=xt[:], scalar=rstd4[i][:], in1=A2[isctx][:],
                                                                               op0=ALU.mult, op1=ALU.mult),
                 reads=[xt, rstd4[i], A2[isctx]], writes=[tmpf])
            S.op("dve", I("tensor_tensor", out=h2f[:], in0=tmpf[:], in1=B2[isctx][:], op=ALU.add),
                 reads=[tmpf, B2[isctx]], writes=[h2f])
            S.op("pool", I("tensor_copy", out=h2b[:], in_=h2f[:]), reads=[h2f], writes=[h2b])
            for c in range(8):
                S.op("pe", I("transpose", out=pT[:, c, :], in_=h2b[:, c * 128:(c + 1) * 128], identity=identb[:]),
                     reads=[h2b, identb], writes=[pT], partial=True, pe_acc=True)
            S.op("act", I("copy", out=H2T[:, :, t * 128:(t + 1) * 128], in_=pT[:]), reads=[pT], writes=[H2T], partial=True)
            for c in range(8):
                S.op("pe", I("transpose", out=ptf[:, c, :], in_=h2f[:, c * 128:(c + 1) * 128], identity=identf[:]),
                     reads=[h2f, identf], writes=[ptf], partial=True, pe_acc=True)
            S.op("act", I("copy", out=h2T32[:], in_=ptf[:]), reads=[ptf], writes=[h2T32])
            for c in range(8):
                S.op("pe", I("matmul", plg[:], lhsT=h2T32[:, c, :], rhs=wrt[:, c, :], start=(c == 0), stop=(c == 7)),
                     reads=[h2T32, wrt], writes=[plg], pe_acc=(c > 0))
            S.op("dve", I("tensor_tensor", out=lgt[:], in0=plg[:], in1=brt[:], op=ALU.add), reads=[plg, brt], writes=[lgt])
            S.op("dve", I("tensor_reduce", out=sm[:, 0:1], in_=lgt[:, 0:4], axis=AX.X, op=ALU.max), reads=[lgt], writes=[sm])
            S.op("dve", I("tensor_scalar", out=ohg[:], in0=lgt[:, 0:4], scalar1=sm[:, 0:1], scalar2=None, op0=ALU.is_equal),
                 reads=[lgt, sm], writes=[ohg])
            S.op("dve", I("tensor_scalar", out=sm[:, 1:2], in0=sm[:, 0:1], scalar1=-1.0, scalar2=None, op0=ALU.mult),
                 reads=[sm], writes=[sm])
            S.op("act", I("activation", out=mk1[:, 0:4], in_=lgt[:, 0:4], func=AF.Exp, bias=sm[:, 1:2], accum_out=sm[:, 2:3]),
                 reads=[lgt, sm], writes=[mk1, sm])
            S.op("dve", I("reciprocal", out=sm[:, 3:4], in_=sm[:, 2:3]), reads=[sm], writes=[sm])
            S.op("dve", I("tensor_scalar", out=ohg[:], in0=ohg[:], scalar1=-1.0, scalar2=1e30, op0=ALU.add, op1=ALU.mult),
                 reads=[ohg], writes=[ohg])
            S.op("dve", I("tensor_tensor", out=lem[:].rearrange("p (g e) -> p g e", e=8),
                                                  in0=lgt[:, 4:36].rearrange("p (g e) -> p g e", e=8),
                                                  in1=ohg[:].unsqueeze(2).broadcast_to([128, 4, 8]), op=ALU.add),
                 reads=[lgt, ohg], writes=[lem])
            S.op("dve", I("max", out=top8[:], in_=lem[:]), reads=[lem], writes=[top8])
            S.op("dve", I("tensor_scalar", out=mk1[:], in0=lem[:], scalar1=top8[:, 0:1], scalar2=None, op0=ALU.is_equal),
                 reads=[lem, top8], writes=[mk1])
            S.op("dve", I("tensor_scalar", out=mk2[:], in0=lem[:], scalar1=top8[:, 1:2], scalar2=None, op0=ALU.is_equal),
                 reads=[lem, top8], writes=[mk2])
            S.op("dve", I("tensor_tensor", out=sm[:, 4:5], in0=top8[:, 1:2], in1=top8[:, 0:1], op=ALU.subtract),
                 reads=[top8, sm], writes=[sm])
            S.op("act", I("activation", out=sm[:, 5:6], in_=sm[:, 4:5], func=AF.Exp), reads=[sm], writes=[sm])
            S.op("dve", I("tensor_scalar", out=sm[:, 6:7], in0=sm[:, 5:6], scalar1=1.0, scalar2=None, op0=ALU.add),
                 reads=[sm], writes=[sm])
            S.op("dve", I("reciprocal", out=sm[:, 7:8], in_=sm[:, 6:7]), reads=[sm], writes=[sm])
            S.op("dve", I("tensor_tensor", out=sm[:, 8:9], in0=sm[:, 7:8], in1=sm[:, 3:4], op=ALU.mult), reads=[sm], writes=[sm])
            S.op("dve", I("tensor_tensor", out=sm[:, 9:10], in0=sm[:, 8:9], in1=sm[:, 5:6], op=ALU.mult), reads=[sm], writes=[sm])
            S.op("dve", I("tensor_scalar", out=mk1[:], in0=mk1[:], scalar1=sm[:, 8:9], scalar2=None, op0=ALU.mult),
                 reads=[mk1, sm], writes=[mk1])
            S.op("dve", I("scalar_tensor_tensor", out=Wgt[:, t, :], in0=mk2[:], scalar=sm[:, 9:10], in1=mk1[:],
                                                              op0=ALU.mult, op1=ALU.add),
                 reads=[mk2, sm, mk1], writes=[Wgt], partial=True)
        S.phase_end()

        S.phase_begin()
        xts = [S.sb([128, D], F32, "xt5") for _ in range(NB)]
        tmpf = S.sb([128, D], F32, "tmpf5")
        junk = tmpf
        rstd4 = [S.sb([128, 1], F32, "rstd5") for _ in range(NB)]
        acc = S.sb([128, TP, D], F32, "acc")
        wgb = S.sb([128, 8, DEXP], BF16, "wgb")
        wub = S.sb([128, 8, DEXP], BF16, "wub")
        wdb = S.sb([128, 4, D], BF16, "wdb")
        st5 = [S.sb([128, 8, 512], F32, "st5") for _ in range(2)]
        pg = [S.ps([128, 512], F32, "pg") for _ in range(1)]
        pu = [S.ps([128, 512], F32, "pu") for _ in range(1)]
        py5 = [S.ps([128, 512], F32, "py5") for _ in range(2)]
        sgs = [S.sb([128, 512], BF16, "sg") for _ in range(2)]
        hid = [S.sb([128, 4, 512], BF16, "hid") for _ in range(2)]
        stc = [0]
        for ex in range(NEXP):
            for (src, dst, shp) in ((wg, wgb, 0), (wu, wub, 0), (wd, wdb, 1)):
                s_ = st5[stc[0] % 2]
                stc[0] += 1
                if shp == 0:
                    S.dma("sp" if stc[0] % 2 else "act", s_[:], src[li, ex].rearrange("(c p) n -> p c n", p=128), reads=[src], writes=[s_])
                    S.op("pool", I("tensor_copy", out=dst[:], in_=s_[:]), reads=[s_], writes=[dst])
                else:
                    S.dma("sp" if stc[0] % 2 else "act", s_[:].rearrange("p c n -> p (c n)").rearrange("p (c n) -> p c n", c=4),
                          src[li, ex].rearrange("(c p) n -> p c n", p=128), reads=[src], writes=[s_])
                    S.op("pool", I("tensor_copy", out=dst[:], in_=s_[:].rearrange("p c n -> p (c n)").rearrange("p (c n) -> p c n", c=4)),
                         reads=[s_], writes=[dst])
            nchunk = (TP + 3) // 4
            for ch in range(nchunk):
                t0 = ch * 4
                ntl = min(4, TP - t0)
                nq = ntl * 128
                hd_ = hid[ch % 2]
                for hb_ in range(4):
                    for c in range(8):
                        S.op("pe", I("matmul", pg[0][:, 0:nq], lhsT=wgb[:, c, hb_ * 128:(hb_ + 1) * 128],
                                                                                 rhs=H2T[:, c, t0 * 128:t0 * 128 + nq], start=(c == 0), stop=(c == 7)),
                             reads=[wgb, H2T], writes=[pg[0]], pe_acc=(c > 0))
                    for c in range(8):
                        S.op("pe", I("matmul", pu[0][:, 0:nq], lhsT=wub[:, c, hb_ * 128:(hb_ + 1) * 128],
                                                                                 rhs=H2T[:, c, t0 * 128:t0 * 128 + nq], start=(c == 0), stop=(c == 7)),
                             reads=[wub, H2T], writes=[pu[0]], pe_acc=(c > 0))
                    sg_ = sgs[hb_ % 2]
                    S.op("act", I("activation", out=sg_[:, 0:nq], in_=pg[0][:, 0:nq], func=AF.Silu),
                         reads=[pg[0]], writes=[sg_])
                    S.op("dve", I("tensor_tensor", out=hd_[:, hb_, 0:nq], in0=sg_[:, 0:nq], in1=pu[0][:, 0:nq], op=ALU.mult),
                         reads=[sg_, pu[0]], writes=[hd_], partial=True)
                for jq in range(ntl):
                    t = t0 + jq
                    for j in range(2):
                        for hb_ in range(4):
                            S.op("pe", I("matmul", py5[j][:], lhsT=hd_[:, hb_, jq * 128:(jq + 1) * 128],
                                                                                      rhs=wdb[:, hb_, j * 512:(j + 1) * 512], start=(hb_ == 0), stop=(hb_ == 3)),
                                 reads=[hd_, wdb], writes=[py5[j]], pe_acc=(hb_ > 0))
                        if ex == 0:
                            S.op("dve", I("tensor_scalar", out=acc[:, t, j * 512:(j + 1) * 512], in0=py5[j][:], scalar1=Wgt[:, t, ex:ex + 1],
                                                                                scalar2=None, op0=ALU.mult), reads=[py5[j], Wgt], writes=[acc], partial=True)
                        else:
                            S.op("dve", I("scalar_tensor_tensor", out=acc[:, t, j * 512:(j + 1) * 512], in0=py5[j][:], scalar=Wgt[:, t, ex:ex + 1],
                                                                                       in1=acc[:, t, j * 512:(j + 1) * 512], op0=ALU.mult, op1=ALU.add),
                                 reads=[py5[j], Wgt, acc], writes=[acc], partial=True)
        G2b = S.sb([128, D], F32, "G2")
        G2 = [G2b, G2b]
        load_mod(0, 5, G2b)
        last = li == cfg.depth - 1
        if last:
            fg = S.sb([128, D], F32, "fg")
            bc_load("sp", fg, fing, fing[0:1, :])
        for t in range(TP):
            i = t % NB
            isctx = 1 if t >= TL else 0
            xt = xts[i]
            if t == TL:
                load_mod(1, 5, G2b)
            S.dma("sp", xt[:], xbuf[t], reads=[xbuf], writes=[xt])
            S.op("dve", I("tensor_tensor", out=tmpf[:], in0=acc[:, t, :], in1=G2[isctx][:], op=ALU.mult),
                 reads=[acc, G2[isctx]], writes=[tmpf])
            S.op("dve", I("tensor_tensor", out=xt[:], in0=xt[:], in1=tmpf[:], op=ALU.add), reads=[xt, tmpf], writes=[xt])
            if not last:
                S.dma("pool", xbuf[t], xt[:], reads=[xt], writes=[xbuf], partial=True)
            else:
                rms_rstd(xt[:], xt, rstd4[i], junk, D)
                S.op("dve", I("scalar_tensor_tensor", out=xt[:], in0=xt[:], scalar=rstd4[i][:], in1=fg[:], op0=ALU.mult, op1=ALU.mult),
                     reads=[xt, rstd4[i], fg], writes=[xt])
                S.dma("pool", out[t], xt[:], reads=[xt], writes=[out], partial=True, is_output=True)
        S.phase_end()
        S.phase_end()
    S.finish()
    return nc, S


def _rope_tables(cfg, core):
    TL, T = cfg.TL, cfg.T
    tok = core * TL * 128 + np.arange(TL * 128)
    pos = np.stack([tok // GRID_W, tok % GRID_W], axis=-1).astype(np.float32)
    quarter = HD // 4
    inv_freq = (1.0 / (10000.0 ** (np.arange(quarter, dtype=np.float32) / quarter))).astype(np.float32)
    ang = pos[:, :, None] * inv_freq
    cos = np.cos(ang).astype(np.float32)
    sin = np.sin(ang).astype(np.float32)
    cosf = np.ones((T * 128, 2, 2, 16), np.float32)
    sinf = np.zeros((T * 128, 2, 2, 16), np.float32)
    cosf[:TL * 128] = cos[:, :, None, :]
    sinf[:TL * 128] = sin[:, :, None, :]
    return cosf.reshape(T, 128, HD), sinf.reshape(T, 128, HD)


def _bias3_table(cfg, core, rpb):
    TL, rows = cfg.TL, cfg.rows
    kh = min(8, rows)
    slots_local = [0, 1, TL - 2, TL - 1, 2]
    outp = np.full((5, 16, 128, 5, 128), NEG, np.float32)
    qq = np.arange(128)
    kk = np.arange(128)
    for s, jl in enumerate(slots_local):
        J = core * TL + jl
        qr = 2 * J + qq // 64
        qc = qq % 64
        r0 = np.clip(qr - kh // 2, 0, rows - kh)
        c0 = np.clip(qc - 8, 0, GRID_W - 16)
        for d in range(5):
            Jk = J + d - 2
            if Jk < 0 or 2 * Jk >= rows:
                continue
            kr = 2 * Jk + kk // 64
            kc = kk % 64
            valid = ((kr[:, None] >= r0[None, :]) & (kr[:, None] < r0[None, :] + kh) &
                     (kc[:, None] >= c0[None, :]) & (kc[:, None] < c0[None, :] + 16))
            dr = np.clip(kr[:, None] - qr[None, :] + 7, 0, 14)
            dc = np.clip(kc[:, None] - qc[None, :] + 15, 0, 30)
            vals = rpb[:, dr, dc]
            outp[s, :, :, d, :] = np.where(valid[None], vals, np.float32(NEG))
    return outp


def _mask1():
    kk = np.arange(128)[:, None]
    qi = np.arange(128)[None, :]
    m = np.zeros((128, 3, 128), np.float32)
    m[:, 0, :] = np.where(kk >= qi, 0.0, NEG)
    m[:, 2, :] = np.where(kk <= qi, 0.0, NEG)
    return m


def make_in_maps(cfg, inp):
    ncore, TL, T = cfg.ncore, cfg.TL, cfg.T
    f = lambda a: np.ascontiguousarray(np.asarray(a, dtype=np.float32))
    x = f(inp["x"])[0]
    ctx = f(inp["ctx"])[0]
    shared = {
        "cc": np.stack([f(inp["c"])[0], f(inp["c_ctx"])], 0),
        "mod_w": f(inp["mod_w"]), "mod_b": f(inp["mod_b"]),
        "n1g": f(inp["norm1_g"]), "n2g": f(inp["norm2_g"]), "fing": f(inp["final_g"])[None, :],
        "wqkv0": f(inp["a_w_qkv"])[0], "wqkv1": f(inp["b_w_qkv"])[0], "wqkv2": f(inp["c_w_qkv"])[0], "wqkv3": f(inp["d_w_qkv"])[0],
        "wo0": f(inp["a_w_o"])[0], "wo1": f(inp["b_w_o"])[0], "wo2": f(inp["c_w_o"])[0], "wo3": f(inp["d_w_o"])[0],
        "a_lam": f(inp["a_lam"])[0], "a_sub": f(inp["a_subln_g"]), "b_sink": f(inp["b_sink"]), "c_qkg": f(inp["c_qk_norm_g"])[0],
        "mask1": _mask1(),
        "wr": np.ascontiguousarray(np.concatenate([f(inp["moe_router_g"]), f(inp["moe_router_e"])], axis=-1)),
        "br": np.ascontiguousarray(np.concatenate([f(inp["moe_router_g_b"]), f(inp["moe_router_e_b"])], axis=-1)),
        "moe_wg": f(inp["moe_w_gate"]), "moe_wu": f(inp["moe_w_up"]), "moe_wd": f(inp["moe_w_down"]),
    }
    rpb = f(inp["d_rpb"])[0]
    maps = []
    for c in range(ncore):
        m = dict(shared)
        xi = np.concatenate([x[c * TL * 128:(c + 1) * TL * 128], ctx], 0).reshape(T, 128, D)
        m["xin"] = np.ascontiguousarray(xi)
        cosf, sinf = _rope_tables(cfg, c)
        m["cosT"], m["sinT"] = cosf, sinf
        sel = np.zeros((128, 2 * ncore), np.float32)
        if c > 0:
            sel[:, c - 1] = 1.0
        if c < ncore - 1:
            sel[:, ncore + c + 1] = 1.0
        m["selT"] = sel
        m["bias3"] = _bias3_table(cfg, c, rpb)
        maps.append(m)
    return maps


_CACHE = {}


def kernel(**inputs):
    cfg = Cfg()
    if "nc" not in _CACHE:
        _CACHE["nc"] = build(cfg)[0]
    nc = _CACHE["nc"]
    in_maps = make_in_maps(cfg, inputs)
    res = run_bass_kernel_spmd(nc, in_maps, core_ids=list(range(cfg.ncore)))
    outs = [np.asarray(r["out"], dtype=np.float32).reshape(cfg.TL * 128, D) for r in res.results]
    return np.concatenate(outs, 0)[None].astype(np.float32)
```

```python
import math
import numpy as np
import concourse.bass as bass
import concourse.mybir as mybir
from concourse.bass_utils import run_bass_kernel_spmd

F32 = mybir.dt.float32
BF16 = mybir.dt.bfloat16
AF = mybir.ActivationFunctionType
ALU = mybir.AluOpType
AX = mybir.AxisListType

D = 1024
HD = 64
GRID_W = 64
CTX = 256
NEXP = 32
DEXP = 512
EPS = 1e-6
NEG = -30000.0


def I(name, *args, **kw):
    return (name, args, kw)


class Buf:
    __slots__ = ("t", "name", "writes", "reads", "dslot", "space")

    def __init__(self, t, name, space):
        self.t = t
        self.name = name
        self.writes = []
        self.reads = []
        self.dslot = {}
        self.space = space

    def __getitem__(self, idx):
        return self.t[idx]


class Sched:
    ENG = ("pe", "dve", "act", "pool", "sp")
    NDSEM = 40

    def __init__(self, nc):
        self.nc = nc
        self.ops = {k: [] for k in self.ENG}
        self.cnt = {k: 0 for k in self.ENG}
        self.semobj = {}
        self.semcnt = {}
        for k in self.ENG:
            self.semobj[("c", k)] = nc.alloc_semaphore("c_" + k)
        self.free_slots = {"hw": [], "sw": []}
        for kind in ("hw", "sw"):
            for i in range(self.NDSEM):
                key = ("d" + kind, i)
                self.semobj[key] = nc.alloc_semaphore("d%s_%d" % (kind, i))
                self.semcnt[key] = 0
                self.free_slots[kind].append(key)
        self.semobj[("cc", 0)] = nc.alloc_semaphore("cc_sem")
        self.semcnt[("cc", 0)] = 0
        self.waited = {k: {} for k in self.ENG}
        self.nbuf = 0
        self.n_inst = 0
        self.out_events = []
        self.phase_bufs = []
        self.marks = []

    def sb(self, shape, dtype, name=None):
        self.nbuf += 1
        name = "%s_%d" % (name or "sb", self.nbuf)
        t = self.nc.alloc_sbuf_tensor(name, list(shape), dtype)
        b = Buf(t, name, "sb")
        self.phase_bufs.append(b)
        return b

    def ps(self, shape, dtype=F32, name=None):
        self.nbuf += 1
        name = "%s_%d" % (name or "ps", self.nbuf)
        t = self.nc.alloc_psum_tensor(name, list(shape), dtype)
        b = Buf(t, name, "ps")
        self.phase_bufs.append(b)
        return b

    def dram(self, name, shape, dtype, kind="Internal"):
        t = self.nc.dram_tensor(name, list(shape), dtype, kind=kind)
        return Buf(t, name, "dram")

    def phase_begin(self):
        nc = self.nc
        self.marks.append((nc.sbuf_base, nc.sbuf_top, nc.psum_base, nc.psum_top, len(self.phase_bufs)))

    def phase_end(self):
        self.barrier()
        nc = self.nc
        sb0, sb1, p0, p1, nb = self.marks.pop()
        for b in self.phase_bufs[nb:]:
            for kind, key in b.dslot.items():
                self.free_slots[kind].append(key)
            b.dslot = {}
        del self.phase_bufs[nb:]
        nc.sbuf_base, nc.sbuf_top = sb0, sb1
        nc.psum_base, nc.psum_top = p0, p1

    def _deps(self, reads, writes):
        deps = []
        for b in reads:
            deps.extend(b.writes)
        for b in writes:
            deps.extend(b.writes)
            deps.extend(b.reads)
        return deps

    def _emit_waits(self, eng, deps, skip_self_pe=False):
        need = {}
        w = self.waited[eng]
        for (key, val) in deps:
            if skip_self_pe and key == ("c", "pe"):
                continue
            if key[0] in ("dhw", "dsw"):
                val = self.semcnt[key]
            if w.get(key, 0) >= val:
                continue
            if need.get(key, 0) < val:
                need[key] = val
        for key, val in need.items():
            self.ops[eng].append(("wait", self.semobj[key], val))
            w[key] = val

    def _commit(self, ev, reads, writes, partial):
        for b in reads:
            b.reads.append(ev)
        for b in writes:
            if partial:
                b.writes.append(ev)
            else:
                b.writes = [ev]
            b.reads = []

    def op(self, eng, fn, reads=(), writes=(), pe_acc=False, partial=False):
        self._emit_waits(eng, self._deps(reads, writes), skip_self_pe=(pe_acc and eng == "pe"))
        self.cnt[eng] += 1
        ev = (("c", eng), self.cnt[eng])
        self.ops[eng].append(("op", fn, self.semobj[("c", eng)]))
        self._commit(ev, reads, writes, partial)
        self.n_inst += 1
        return ev

    def dma(self, q, out_ap, in_ap, reads=(), writes=(), sembuf=None, partial=False, is_output=False):
        self._emit_waits(q, self._deps(reads, writes))
        if sembuf is None:
            cands = [b for b in list(writes) + list(reads) if b.space == "sb"]
            sembuf = cands[0] if cands else (list(writes) + list(reads))[0]
        kind = "sw" if q == "pool" else "hw"
        if kind not in sembuf.dslot:
            sembuf.dslot[kind] = self.free_slots[kind].pop(0)
            if sembuf.space == "dram" and sembuf not in self.phase_bufs:
                self.phase_bufs.append(sembuf)
        key = sembuf.dslot[kind]
        self.semcnt[key] += 16
        ev = (key, self.semcnt[key])
        self.ops[q].append(("dma", out_ap, in_ap, self.semobj[key]))
        self._commit(ev, reads, writes, partial)
        if is_output:
            self.out_events.append(ev)
        self.n_inst += 1
        return ev

    def collective(self, kind, in_ap, out_ap, groups, reads=(), writes=()):
        self._emit_waits("pool", self._deps(reads, writes))
        key = ("cc", 0)
        self.semcnt[key] += 1
        ev = (key, self.semcnt[key])
        self.ops["pool"].append(("cc", kind, in_ap, out_ap, groups, self.semobj[key]))
        self._commit(ev, reads, writes, False)
        return ev

    def barrier(self):
        evs = [(("c", k), self.cnt[k]) for k in self.ENG]
        for key, v in self.semcnt.items():
            if v > 0:
                evs.append((key, v))
        for k in self.ENG:
            self._emit_waits(k, [e for e in evs if e[0] != ("c", k)])

    def finish(self):
        self._emit_waits("sp", self.out_events)
        self.barrier()
        nc = self.nc
        allsems = list(self.semobj.values())
        with nc.Block() as block0:
            def clr(engine):
                for s_ in allsems:
                    engine.sem_clear(s_)
            block0.gpsimd(clr)
        with nc.Block() as block:
            for k, deco in (("pe", block.tensor), ("dve", block.vector), ("act", block.scalar),
                            ("pool", block.gpsimd), ("sp", block.sync)):
                ops = self.ops[k]

                def body(engine, ops=ops):
                    for o in ops:
                        if o[0] == "wait":
                            engine.wait_ge(o[1], o[2])
                        elif o[0] == "op":
                            nm, a_, kw_ = o[1]
                            getattr(engine, nm)(*a_, **kw_).then_inc(o[2], 1)
                        elif o[0] == "dma":
                            engine.dma_start(out=o[1], in_=o[2]).then_inc(o[3], 16)
                        else:
                            engine.collective_compute(o[1], ALU.bypass, replica_groups=o[4],
                                                      ins=[o[2]], outs=[o[3]]).then_inc(o[5], 1)
                deco(body)
        with nc.Block() as block2:
            def clr2(engine):
                for s_ in allsems:
                    engine.sem_clear(s_)
            block2.gpsimd(clr2)
        return nc


class Cfg:
    def __init__(self, ncore=8, TL=16, depth=4):
        self.ncore = ncore
        self.TL = TL
        self.TC = CTX // 128
        self.T = TL + self.TC
        self.SEQ = ncore * TL * 128
        self.rows = self.SEQ // GRID_W
        self.depth = depth


MIX = {0: (3072, 16, 8, 128), 1: (1536, 4, 4, 64), 2: (1536, 4, 4, 64), 3: (3072, 16, 16, 64)}


def build(cfg):
    nc = bass.Bass("TRN2", target_bir_lowering=False)
    S = Sched(nc)
    ncore, TL, TC, T = cfg.ncore, cfg.TL, cfg.TC, cfg.T
    NTOK = T * 128
    groups = [list(range(ncore))]

    def ext(name, shape, dt=F32):
        return S.dram(name, shape, dt, kind="ExternalInput")

    xin = ext("xin", [T, 128, D])
    cc = ext("cc", [2, D])
    mod_w = ext("mod_w", [4, D, 6 * D])
    mod_b = ext("mod_b", [4, 6 * D])
    n1g = ext("n1g", [4, D])
    n2g = ext("n2g", [4, D])
    fing = ext("fing", [1, D])
    wqkv = [ext("wqkv%d" % m, [D, MIX[m][0]]) for m in range(4)]
    wo = [ext("wo%d" % m, [D, D]) for m in range(4)]
    a_lam = ext("a_lam", [4, HD])
    a_sub = ext("a_sub", [1, 128])
    b_sink = ext("b_sink", [1, 16])
    c_qkg = ext("c_qkg", [2, HD])
    bias3 = ext("bias3", [5, 16, 128, 5, 128])
    mask1 = ext("mask1", [128, 3, 128])
    cosT = ext("cosT", [T, 128, HD])
    sinT = ext("sinT", [T, 128, HD])
    selT = ext("selT", [128, 2 * ncore])
    wr = ext("wr", [4, D, 36])
    br = ext("br", [4, 36])
    wg = ext("moe_wg", [4, NEXP, D, DEXP])
    wu = ext("moe_wu", [4, NEXP, D, DEXP])
    wd = ext("moe_wd", [4, NEXP, DEXP, D])
    out = S.dram("out", [TL, 128, D], F32, kind="ExternalOutput")

    xbuf = S.dram("xbuf", [T, 128, D], F32)
    modr = S.dram("modr", [2, 6 * D], F32)
    qt_d = S.dram("qt_d", [16, HD, NTOK], BF16)
    NSRCS = {m_: MIX[m_][1] * HD * TL * 128 + MIX[m_][2] * 128 * TL * (MIX[m_][3] + 1) for m_ in range(4)}
    kvsrcs = {m_: S.dram("kvsrc%d" % m_, [1, NSRCS[m_]], BF16) for m_ in range(4)}
    kvgs = {m_: S.dram("kvg%d" % m_, [ncore, NSRCS[m_]], BF16) for m_ in (0, 2)}
    ao_d = S.dram("ao_d", [T, 128, D], BF16)
    kc_d = S.dram("kc_d", [16, HD, CTX], BF16)
    vc_d = S.dram("vc_d", [16 * 128 * TC * 129], BF16)
    HWS = {1: 1, 3: 2}
    HKS = {m_: MIX[m_][1] * HD * HWS[m_] * 128 for m_ in (1, 3)}
    HVS = {m_: MIX[m_][2] * 128 * HWS[m_] * (MIX[m_][3] + 1) for m_ in (1, 3)}
    hsrcs = {m_: S.dram("hsrc%d" % m_, [1, 2 * (HKS[m_] + HVS[m_])], BF16) for m_ in (1, 3)}
    hgs = {m_: S.dram("hg%d" % m_, [ncore, 2 * (HKS[m_] + HVS[m_])], BF16) for m_ in (1, 3)}
    kext = S.dram("kext", [16 * HD * (TL + 4) * 128], BF16)
    vext = S.dram("vext", [16 * 128 * (TL + 4) * 65], BF16)

    def dview(buf, off, pattern):
        return bass.AP(buf.t, off, pattern)

    identf = S.sb([128, 128], F32, "identf")
    identb = S.sb([128, 128], BF16, "identb")
    S.op("pool", I("memset", identf[:], 0.0), writes=[identf])
    S.op("pool", I("affine_select", out=identf[:], in_=identf[:], pattern=[[-1, 128]],
                                           compare_op=ALU.not_equal, fill=1.0, base=0, channel_multiplier=1),
         reads=[identf], writes=[identf])
    S.op("dve", I("tensor_copy", out=identb[:], in_=identf[:]), reads=[identf], writes=[identb])
    zer = S.sb([128, 512], BF16, "zer")
    S.op("pool", I("memset", zer[:], 0.0), writes=[zer])

    S.phase_begin()
    for t in range(T):
        xt = S.sb([128, D], F32, "xcp")
        S.dma("sp", xt[:], xin[t], reads=[xin], writes=[xt])
        S.dma("sp", xbuf[t], xt[:], reads=[xt], writes=[xbuf], partial=True)
    S.phase_end()

    def bc_load(q, dst, src_buf, row_ap):
        S.dma(q, dst[:], row_ap.partition_broadcast(128), reads=[src_buf], writes=[dst])

    def rms_rstd(xt_ap, xt_buf, rstd, junk, n):
        ssb = S.sb([128, 1], F32, "ss")
        S.op("act", I("activation", out=junk[:], in_=xt_ap, func=AF.Square, accum_out=ssb[:]),
             reads=[xt_buf], writes=[junk, ssb])
        S.op("dve", I("tensor_scalar", out=rstd[:], in0=ssb[:], scalar1=1.0 / n, scalar2=EPS,
                                              op0=ALU.mult, op1=ALU.add), reads=[ssb], writes=[rstd])
        S.op("act", I("activation", out=rstd[:], in_=rstd[:], func=AF.Sqrt), reads=[rstd], writes=[rstd])
        S.op("dve", I("reciprocal", out=rstd[:], in_=rstd[:]), reads=[rstd], writes=[rstd])

    for li in range(cfg.depth):
        mixer = li % 4
        ncols, nk, nv, dv = MIX[mixer]
        dv1 = dv + 1
        need_ctx = li < cfg.depth - 1
        local = mixer in (1, 3)
        hw = 1 if mixer == 1 else 2
        KOFF = 0
        VOFF = nk * HD * TL * 128
        lam_init = 0.8 - 0.6 * math.exp(-0.3 * li)
        kvsrc = kvsrcs[mixer]
        kvg = kvgs.get(mixer)
        NSRC = NSRCS[mixer]
        if local:
            HK, HV = HKS[mixer], HVS[mixer]
            HS = 2 * (HK + HV)
            hsrc, hg = hsrcs[mixer], hgs[mixer]

        S.phase_begin()
        cct = S.sb([2, D], F32, "cct")
        S.dma("sp", cct[:], cc[:], reads=[cc], writes=[cct])
        S.op("act", I("activation", out=cct[:], in_=cct[:], func=AF.Silu), reads=[cct], writes=[cct])
        psT = S.ps([128, 8, 2], F32, "psT")
        for c in range(8):
            S.op("pe", I("transpose", out=psT[:, c, :], in_=cct[:, c * 128:(c + 1) * 128],
                                                  identity=identf[0:2, 0:2]),
                 reads=[cct, identf], writes=[psT], partial=True, pe_acc=True)
        sT = S.sb([128, 8, 2], F32, "sT")
        S.op("dve", I("tensor_copy", out=sT[:], in_=psT[:]), reads=[psT], writes=[sT])
        mrow = S.sb([2, 6 * D], F32, "mrow")
        mbt = S.sb([2, 6 * D], F32, "mbt")
        S.dma("sp", mbt[:], dview(mod_b, li * 6 * D, [[0, 2], [1, 6 * D]]), reads=[mod_b], writes=[mbt])
        mwb = [S.sb([128, 8, 512], F32, "mw") for _ in range(2)]
        pm = [S.ps([2, 512], F32, "pm") for _ in range(2)]
        for j in range(12):
            w_ = mwb[j % 2]
            S.dma("sp" if j % 2 == 0 else "act", w_[:],
                  mod_w[li, :, j * 512:(j + 1) * 512].rearrange("(c p) n -> p c n", p=128),
                  reads=[mod_w], writes=[w_])
            p_ = pm[j % 2]
            for c in range(8):
                S.op("pe", I("matmul", p_[:], lhsT=sT[:, c, :], rhs=w_[:, c, :],
                                                                 start=(c == 0), stop=(c == 7)),
                     reads=[sT, w_], writes=[p_], pe_acc=(c > 0))
            S.op("dve", I("tensor_tensor", out=mrow[:, j * 512:(j + 1) * 512], in0=p_[:],
                                                              in1=mbt[:, j * 512:(j + 1) * 512], op=ALU.add),
                 reads=[p_, mbt], writes=[mrow], partial=True)
        S.dma("sp", modr[:], mrow[:], reads=[mrow], writes=[modr])
        S.phase_end()

        def load_mod(which, vec, dst, q="sp"):
            bc_load(q, dst, modr, modr[which:which + 1, vec * D:(vec + 1) * D])

        S.phase_begin()
        gbc = S.sb([128, D], F32, "gbc")
        bc_load("sp", gbc, n1g, n1g[li:li + 1, :])
        A1 = [S.sb([128, D], F32, "A1") for _ in range(2)]
        B1 = [S.sb([128, D], F32, "B1") for _ in range(2)]
        for w_ in range(2):
            load_mod(w_, 1, A1[w_])
            S.op("dve", I("scalar_tensor_tensor", out=A1[w_][:], in0=A1[w_][:], scalar=1.0, in1=gbc[:],
                                                                op0=ALU.add, op1=ALU.mult),
                 reads=[A1[w_], gbc], writes=[A1[w_]])
            load_mod(w_, 0, B1[w_])
        wq = S.sb([128, 8, ncols], BF16, "wq")
        wst = [S.sb([128, 8, 512], F32, "wst") for _ in range(2)]
        for j in range(ncols // 512):
            s_ = wst[j % 2]
            S.dma("sp" if j % 2 == 0 else "act", s_[:],
                  wqkv[mixer][:, j * 512:(j + 1) * 512].rearrange("(c p) n -> p c n", p=128),
                  reads=[wqkv[mixer]], writes=[s_])
            S.op("pool", I("tensor_copy", out=wq[:, :, j * 512:(j + 1) * 512], in_=s_[:]),
                 reads=[s_], writes=[wq], partial=True)
        if mixer == 2:
            qg = S.sb([128, HD], F32, "qg")
            kg = S.sb([128, HD], F32, "kg")
            bc_load("sp", qg, c_qkg, c_qkg[0:1, :])
            bc_load("sp", kg, c_qkg, c_qkg[1:2, :])
        NB = 2
        xts = [S.sb([128, D], F32, "xt") for _ in range(NB)]
        junk = S.sb([128, D], F32, "junk")
        tmpf = S.sb([128, D], F32, "tmpf")
        hbs = [S.sb([128, D], BF16, "hb") for _ in range(NB)]
        hTs = [S.sb([128, 8, 128], BF16, "hT") for _ in range(NB)]
        pTs = [S.ps([128, 8, 128], BF16, "pT") for _ in range(NB)]
        pq = [S.ps([128, 512], F32, "pq") for _ in range(2)]
        qkvs = [S.sb([128, ncols], F32, "qkv") for _ in range(NB)]
        cosb = [S.sb([128, HD], F32, "cos") for _ in range(NB)]
        sinb = [S.sb([128, HD], F32, "sin") for _ in range(NB)]
        ra = S.sb([128, 16 * 32], F32, "ra")
        rb = S.sb([128, 16 * 32], F32, "rb")
        sq = S.sb([128, 16 * HD], F32, "sq")
        qb = [S.sb([128, 16, HD], BF16, "qb") for _ in range(NB)]
        kb = [S.sb([128, nk, HD], BF16, "kb") for _ in range(NB)]
        va = [S.sb([128, nv, dv1], BF16, "va") for _ in range(NB)]
        for v_ in va:
            S.op("pool", I("memset", v_[:], 1.0), writes=[v_])
        pqT = [S.ps([HD, 16, 128], BF16, "pqT") for _ in range(1)]
        qTs = [S.sb([HD, 16, 128], BF16, "qTs") for _ in range(NB)]
        kTs = [S.sb([HD, nk, 128], BF16, "kTs") for _ in range(NB)]
        rstds = [S.sb([128, 1], F32, "rstd") for _ in range(NB)]
        nrm = S.sb([128, 16], F32, "nrm")

        def rope(src_buf, src_ap, dst_buf, dst_ap, nh, cb, sb_):
            sv = src_ap.rearrange("p (h a b d) -> p h a b d", a=2, b=2, d=16)
            dvw = dst_ap.rearrange("p h (a b d) -> p h a b d", a=2, b=2, d=16)
            cv = cb[:].rearrange("p (a b d) -> p a b d", a=2, b=2)[:, :, 0, :].unsqueeze(1).broadcast_to([128, nh, 2, 16])
            sn = sb_[:].rearrange("p (a b d) -> p a b d", a=2, b=2)[:, :, 0, :].unsqueeze(1).broadcast_to([128, nh, 2, 16])
            x1 = sv[:, :, :, 0, :]
            x2 = sv[:, :, :, 1, :]
            rav = ra[:, 0:nh * 32].rearrange("p (h a d) -> p h a d", a=2, d=16)
            rbv = rb[:, 0:nh * 32].rearrange("p (h a d) -> p h a d", a=2, d=16)
            S.op("dve", I("tensor_tensor", out=rav, in0=x1, in1=cv, op=ALU.mult), reads=[src_buf, cb], writes=[ra])
            S.op("dve", I("tensor_tensor", out=rbv, in0=x2, in1=sn, op=ALU.mult), reads=[src_buf, sb_], writes=[rb])
            S.op("dve", I("tensor_tensor", out=dvw[:, :, :, 0, :], in0=rav, in1=rbv, op=ALU.subtract),
                 reads=[ra, rb], writes=[dst_buf], partial=True)
            S.op("dve", I("tensor_tensor", out=rav, in0=x2, in1=cv, op=ALU.mult), reads=[src_buf, cb], writes=[ra])
            S.op("dve", I("tensor_tensor", out=rbv, in0=x1, in1=sn, op=ALU.mult), reads=[src_buf, sb_], writes=[rb])
            S.op("dve", I("tensor_tensor", out=dvw[:, :, :, 1, :], in0=rav, in1=rbv, op=ALU.add),
                 reads=[ra, rb], writes=[dst_buf], partial=True)

        def headnorm(buf, ap, nh, gtile):
            v3 = ap.rearrange("p (h d) -> p h d", d=HD)
            sq3 = sq[:, 0:nh * HD].rearrange("p (h d) -> p h d", d=HD)
            S.op("dve", I("tensor_tensor", out=sq3, in0=v3, in1=v3, op=ALU.mult), reads=[buf], writes=[sq])
            S.op("dve", I("tensor_reduce", out=nrm[:, 0:nh], in_=sq3, axis=AX.X, op=ALU.add), reads=[sq], writes=[nrm])
            S.op("dve", I("tensor_scalar", out=nrm[:, 0:nh], in0=nrm[:, 0:nh], scalar1=1.0 / HD, scalar2=EPS,
                                                  op0=ALU.mult, op1=ALU.add), reads=[nrm], writes=[nrm])
            S.op("act", I("activation", out=nrm[:, 0:nh], in_=nrm[:, 0:nh], func=AF.Sqrt), reads=[nrm], writes=[nrm])
            S.op("dve", I("reciprocal", out=nrm[:, 0:nh], in_=nrm[:, 0:nh]), reads=[nrm], writes=[nrm])
            S.op("dve", I("tensor_tensor", out=v3, in0=v3, in1=nrm[:, 0:nh].unsqueeze(2).broadcast_to([128, nh, HD]),
                                                  op=ALU.mult), reads=[buf, nrm], writes=[buf])
            S.op("dve", I("tensor_tensor", out=v3, in0=v3, in1=gtile[:].unsqueeze(1).broadcast_to([128, nh, HD]),
                                                  op=ALU.mult), reads=[buf, gtile], writes=[buf])

        for t in range(T):
            i = t % NB
            isctx = 1 if t >= TL else 0
            xt, hb, hT, pT, qkv = xts[i], hbs[i], hTs[i], pTs[i], qkvs[i]
            S.dma("sp", xt[:], xbuf[t], reads=[xbuf], writes=[xt])
            if mixer != 3:
                S.dma("act", cosb[i][:], cosT[t], reads=[cosT], writes=[cosb[i]])
                S.dma("act", sinb[i][:], sinT[t], reads=[sinT], writes=[sinb[i]])
            rms_rstd(xt[:], xt, rstds[i], junk, D)
            S.op("dve", I("scalar_tensor_tensor",
                out=tmpf[:], in0=xt[:], scalar=rstds[i][:], in1=A1[isctx][:], op0=ALU.mult, op1=ALU.mult),
                reads=[xt, rstds[i], A1[isctx]], writes=[tmpf])
            S.op("dve", I("tensor_tensor", out=hb[:], in0=tmpf[:], in1=B1[isctx][:], op=ALU.add),
                 reads=[tmpf, B1[isctx]], writes=[hb])
            for c in range(8):
                S.op("pe", I("transpose", out=pT[:, c, :], in_=hb[:, c * 128:(c + 1) * 128],
                                                                   identity=identb[:]),
                     reads=[hb, identb], writes=[pT], partial=True, pe_acc=True)
            S.op("act", I("copy", out=hT[:], in_=pT[:]), reads=[pT], writes=[hT])
            for j in range(ncols // 512):
                p_ = pq[j % 2]
                for c in range(8):
                    S.op("pe", I("matmul", p_[:], lhsT=hT[:, c, :],
                                                                         rhs=wq[:, c, j * 512:(j + 1) * 512],
                                                                         start=(c == 0), stop=(c == 7)),
                         reads=[hT, wq], writes=[p_], pe_acc=(c > 0))
                S.op("act", I("copy", out=qkv[:, j * 512:(j + 1) * 512], in_=p_[:]),
                     reads=[p_], writes=[qkv], partial=True)
            qap = qkv[:, 0:1024]
            kap = qkv[:, 1024:1024 + nk * HD]
            vap = qkv[:, 1024 + nk * HD:ncols]
            if mixer == 2:
                headnorm(qkv, qap, 16, qg)
                headnorm(qkv, kap, nk, kg)
            if mixer != 3:
                rope(qkv, qap, qb[i], qb[i][:], 16, cosb[i], sinb[i])
                rope(qkv, kap, kb[i], kb[i][:], nk, cosb[i], sinb[i])
            else:
                S.op("dve", I("tensor_copy", out=qb[i][:], in_=qap.rearrange("p (h d) -> p h d", d=HD)),
                     reads=[qkv], writes=[qb[i]])
                S.op("dve", I("tensor_copy", out=kb[i][:], in_=kap.rearrange("p (h d) -> p h d", d=HD)),
                     reads=[qkv], writes=[kb[i]])
            S.op("pool", I("tensor_copy", out=va[i][:, :, 0:dv], in_=vap.rearrange("p (h d) -> p h d", d=dv)),
                 reads=[qkv], writes=[va[i]])
            pqt = pqT[0]
            for h in range(16):
                S.op("pe", I("transpose", out=pqt[:, h, :], in_=qb[i][:, h, :], identity=identb[:]),
                     reads=[qb[i], identb], writes=[pqt], partial=True, pe_acc=True)
            S.op("act", I("copy", out=qTs[i][:], in_=pqt[:]), reads=[pqt], writes=[qTs[i]])
            S.dma("pool", qt_d[:, :, t * 128:(t + 1) * 128].rearrange("h d n -> d h n"), qTs[i][:],
                  reads=[qTs[i]], writes=[qt_d], partial=True)
            for h in range(nk):
                S.op("pe", I("transpose", out=pqt[:, h, :], in_=kb[i][:, h, :], identity=identb[:]),
                     reads=[kb[i], identb], writes=[pqt], partial=True, pe_acc=True)
            S.op("act", I("copy", out=kTs[i][:], in_=pqt[:, 0:nk, :]), reads=[pqt], writes=[kTs[i]])
            if not isctx:
                kdst = dview(kvsrc, KOFF + t * 128, [[TL * 128, HD], [HD * TL * 128, nk], [1, 128]])
                vdst = dview(kvsrc, VOFF + t * dv1, [[TL * dv1, 128], [128 * TL * dv1, nv], [1, dv1]])
                S.dma("pool", kdst, kTs[i][:], reads=[kTs[i]], writes=[kvsrc], partial=True)
                S.dma("pool", vdst, va[i][:], reads=[va[i]], writes=[kvsrc], partial=True)
                if local and (t < hw or t >= TL - hw):
                    which = 0 if t < hw else 1
                    tt = t if t < hw else t - (TL - hw)
                    hoff = which * (HK + HV)
                    kd2 = dview(hsrc, hoff + tt * 128, [[hw * 128, HD], [HD * hw * 128, nk], [1, 128]])
                    vd2 = dview(hsrc, hoff + HK + tt * dv1, [[hw * dv1, 128], [128 * hw * dv1, nv], [1, dv1]])
                    S.dma("pool", kd2, kTs[i][:], reads=[kTs[i]], writes=[hsrc], partial=True)
                    S.dma("pool", vd2, va[i][:], reads=[va[i]], writes=[hsrc], partial=True)
            else:
                tt = t - TL
                kdst = dview(kc_d, tt * 128, [[CTX, HD], [HD * CTX, nk], [1, 128]])
                vdst = dview(vc_d, tt * dv1, [[TC * dv1, 128], [128 * TC * dv1, nv], [1, dv1]])
                S.dma("pool", kdst, kTs[i][:], reads=[kTs[i]], writes=[kc_d], partial=True)
                S.dma("pool", vdst, va[i][:], reads=[va[i]], writes=[vc_d], partial=True)
        S.phase_end()

        S.phase_begin()
        if not local:
            S.collective("AllGather", kvsrc.t.ap().opt(), kvg.t.ap().opt(), groups, reads=[kvsrc], writes=[kvg])
            NL = ncore * TL
        else:
            NL = TL + 2 * hw
            S.collective("AllGather", hsrc.t.ap().opt(), hg.t.ap().opt(), groups, reads=[hsrc], writes=[hg])
            selb = S.sb([128, 2 * ncore], F32, "selb")
            S.dma("sp", selb[:], selT[:], reads=[selT], writes=[selb])
            S.dma("sp", dview(kext, hw * 128, [[NL * 128, nk * HD], [1, TL * 128]]),
                  dview(kvsrc, KOFF, [[TL * 128, nk * HD], [1, TL * 128]]), reads=[kvsrc], writes=[kext], partial=True)
            S.dma("act", dview(vext, hw * dv1, [[NL * dv1, nv * 128], [1, TL * dv1]]),
                  dview(kvsrc, VOFF, [[TL * dv1, nv * 128], [1, TL * dv1]]), reads=[kvsrc], writes=[vext], partial=True)
            for side in range(2):
                which = 1 if side == 0 else 0
                hoff = which * (HK + HV)
                kacc = S.sb([HD, nk, hw * 128], BF16, "kacc")
                vacc = S.sb([128, nv, hw * dv1], BF16, "vacc")
                S.op("pool", I("memset", kacc[:], 0.0), writes=[kacc])
                S.op("pool", I("memset", vacc[:], 0.0), writes=[vacc])
                kin = [S.sb([HD, nk, hw * 128], BF16, "kin") for _ in range(2)]
                vin = [S.sb([128, nv, hw * dv1], BF16, "vin") for _ in range(2)]
                for r in range(ncore):
                    k_, v_ = kin[r % 2], vin[r % 2]
                    S.dma("sp", k_[:], dview(hg, r * HS + hoff, [[hw * 128, HD], [HD * hw * 128, nk], [1, hw * 128]]),
                          reads=[hg], writes=[k_])
                    S.dma("act", v_[:], dview(hg, r * HS + hoff + HK, [[hw * dv1, 128], [128 * hw * dv1, nv], [1, hw * dv1]]),
                          reads=[hg], writes=[v_])
                    col = side * ncore + r
                    S.op("dve", I("scalar_tensor_tensor",
                        out=kacc[:], in0=k_[:], scalar=selb[0:HD, col:col + 1], in1=kacc[:], op0=ALU.mult, op1=ALU.add),
                        reads=[k_, selb, kacc], writes=[kacc])
                    S.op("dve", I("scalar_tensor_tensor",
                        out=vacc[:], in0=v_[:], scalar=selb[:, col:col + 1], in1=vacc[:], op0=ALU.mult, op1=ALU.add),
                        reads=[v_, selb, vacc], writes=[vacc])
                toff = 0 if side == 0 else hw + TL
                S.dma("sp", dview(kext, toff * 128, [[NL * 128, HD], [HD * NL * 128, nk], [1, hw * 128]]), kacc[:],
                      reads=[kacc], writes=[kext], partial=True)
                S.dma("sp", dview(vext, toff * dv1, [[NL * dv1, 128], [128 * NL * dv1, nv], [1, hw * dv1]]), vacc[:],
                      reads=[vacc], writes=[vext], partial=True)
        S.phase_end()

        S.phase_begin()
        NKT = TC + NL
        aost = [S.sb([128, 128], BF16, "aost") for _ in range(4)]
        aoc = [0]
        chc = [0]
        ngroup = 8 if mixer == 0 else 16
        gsz = 2 if mixer == 0 else 1
        KT = [S.sb([HD, NKT * 128], BF16, "KT") for _ in range(2)]
        VT = [S.sb([128, NKT, dv1], BF16, "VT") for _ in range(2)]
        QT = [S.sb([HD, NTOK], BF16, "QT") for _ in range(2 * gsz)]
        NPS = 3
        pS = [S.ps([128, 512], F32, "pS") for _ in range(NPS)]
        nacc = 2 if dv1 > 128 else 1
        nset = 1 if mixer == 0 else 2
        pA = [[S.ps([128, 4 // nacc, dv1], F32, "pA") for _ in range(nacc)] for _ in range(nset * gsz)]
        PTs = [S.sb([128, 512], BF16, "PT") for _ in range(4)]
        sbias = [S.sb([128, 512], F32, "sbias") for _ in range(2)]
        if mixer == 1:
            m1 = S.sb([128, 3, 128], F32, "m1")
            S.dma("sp", m1[:], mask1[:], reads=[mask1], writes=[m1])
            sk = S.sb([128, 16], F32, "sk")
            bc_load("sp", sk, b_sink, b_sink[0:1, :])
            S.op("act", I("activation", out=sk[:], in_=sk[:], func=AF.Exp), reads=[sk], writes=[sk])
        if mixer == 3:
            b3 = [S.sb([128, 5, 128], F32, "b3") for _ in range(2)]
        if mixer == 0:
            lamt = S.sb([128, 4, HD], F32, "lamt")
            S.dma("sp", lamt[:], dview(a_lam, 0, [[0, 128], [1, 4 * HD]]), reads=[a_lam], writes=[lamt])
            lp = S.sb([128, 2, HD], F32, "lp")
            S.op("dve", I("tensor_tensor", out=lp[:, 0, :], in0=lamt[:, 0, :], in1=lamt[:, 1, :], op=ALU.mult),
                 reads=[lamt], writes=[lp], partial=True)
            S.op("dve", I("tensor_tensor", out=lp[:, 1, :], in0=lamt[:, 2, :], in1=lamt[:, 3, :], op=ALU.mult),
                 reads=[lamt], writes=[lp], partial=True)
            ls = S.sb([128, 2], F32, "ls")
            S.op("dve", I("tensor_reduce", out=ls[:], in_=lp[:], axis=AX.X, op=ALU.add), reads=[lp], writes=[ls])
            S.op("act", I("activation", out=ls[:], in_=ls[:], func=AF.Exp), reads=[ls], writes=[ls])
            nlam = S.sb([128, 1], F32, "nlam")
            S.op("dve", I("tensor_tensor", out=nlam[:], in0=ls[:, 1:2], in1=ls[:, 0:1], op=ALU.subtract),
                 reads=[ls], writes=[nlam])
            S.op("dve", I("tensor_scalar", out=nlam[:], in0=nlam[:], scalar1=-lam_init, scalar2=None, op0=ALU.add),
                 reads=[nlam], writes=[nlam])
            subg = S.sb([128, 128], F32, "subg")
            bc_load("sp", subg, a_sub, a_sub[0:1, :])
            S.op("dve", I("tensor_scalar", out=subg[:], in0=subg[:], scalar1=1.0 - lam_init, scalar2=None, op0=ALU.mult),
                 reads=[subg], writes=[subg])
        o0 = S.sb([128, 129], F32, "o0")
        o1 = S.sb([128, 129], F32, "o1")
        rs_ = S.sb([128, 2], F32, "rs")
        jk = S.sb([128, 128], F32, "jk")
        ss1 = S.sb([128, 1], F32, "ss1")
        ptc = [0]
        psc = [0]

        def kidx_of(m):
            return m if mixer in (0, 3) else m // 4

        def vidx_of(g):
            return g if mixer in (0, 3) else g // 4

        last_k = {}
        last_v = [None, None]
        kslot = [0]
        vslot = [0]

        def load_k(kidx):
            if kidx in last_k:
                return last_k[kidx]
            b = KT[kslot[0] % 2]
            kslot[0] += 1
            for kk in [k_ for k_, v_ in last_k.items() if v_ is b]:
                del last_k[kk]
            S.dma("sp", b[:, 0:CTX], kc_d[kidx], reads=[kc_d], writes=[b])
            if local:
                S.dma("sp", b[:, CTX:NKT * 128], dview(kext, kidx * HD * NL * 128, [[NL * 128, HD], [1, NL * 128]]),
                      reads=[kext], writes=[b], partial=True)
            else:
                src = dview(kvg, KOFF + kidx * HD * TL * 128, [[TL * 128, HD], [NSRC, ncore], [1, TL * 128]])
                S.dma("sp", b[:, CTX:NKT * 128].rearrange("d (r n) -> d r n", r=ncore), src, reads=[kvg], writes=[b], partial=True)
            last_k[kidx] = b
            return b

        def load_v(vidx):
            if last_v[0] == vidx:
                return last_v[1]
            b = VT[vslot[0] % 2]
            vslot[0] += 1
            S.dma("act", b[:, 0:TC, :], dview(vc_d, vidx * 128 * TC * dv1, [[TC * dv1, 128], [dv1, TC], [1, dv1]]),
                  reads=[vc_d], writes=[b])
            if local:
                S.dma("act", b[:, TC:NKT, :], dview(vext, vidx * 128 * NL * dv1, [[NL * dv1, 128], [dv1, NL], [1, dv1]]),
                      reads=[vext], writes=[b], partial=True)
            else:
                src = dview(kvg, VOFF + vidx * 128 * TL * dv1, [[TL * dv1, 128], [NSRC, ncore], [1, TL * dv1]])
                S.dma("act", b[:, TC:NKT, :].rearrange("p (r t) e -> p r (t e)", r=ncore), src, reads=[kvg], writes=[b], partial=True)
            last_v[0], last_v[1] = vidx, b
            return b

        for g in range(ngroup):
            maps = [g * gsz + a for a in range(gsz)]
            Vb = load_v(vidx_of(g))
            Kbs = [load_k(kidx_of(m)) for m in maps]
            Qbs = []
            for a, m in enumerate(maps):
                qb_ = QT[(g % 2) * gsz + a]
                S.dma("pool", qb_[:], qt_d[m], reads=[qt_d], writes=[qb_])
                Qbs.append(qb_)
            chunks = []
            if local:
                for j in range(TL):
                    keys = [(kt, None) for kt in range(TC)]
                    for d in range(2 * hw + 1):
                        keys.append((TC + j + d, d))
                    chunks.append((j, 1, keys))
            else:
                for j in range(0, TL, 4):
                    chunks.append((j, min(4, TL - j), [(kt, None) for kt in range(NKT)]))
            if need_ctx:
                chunks.append((TL, TC, [(kt, None) for kt in range(TC)]))
            for (t0, ntl, keys) in chunks:
                nq = ntl * 128
                if mixer == 3 and t0 < TL:
                    slot = t0 if t0 < 2 else (2 + t0 - (TL - 2) if t0 >= TL - 2 else 4)
                    bb = b3[t0 % 2]
                    S.dma("sp", bb[:], bias3[slot, g], reads=[bias3], writes=[bb])
                chc[0] += 1
                aset = chc[0] % nset
                for a, m in enumerate(maps):
                    accs = pA[aset * gsz + a]
                    kb_, qb__ = Kbs[a], Qbs[a]
                    for ab in accs:
                        ncol = (4 // nacc) * dv1
                        S.op("pe", I("matmul", ab[:].rearrange("p a e -> p (a e)"),
                                                                      lhsT=zer[:, 0:128], rhs=zer[:, 0:ncol],
                                                                      start=True, stop=True, skip_group_check=True),
                             reads=[zer], writes=[ab])
                    def emit_mm1(ki, kb_=kb_, qb__=qb__, t0=t0, nq=nq):
                        kt, bd = keys[ki]
                        ps_ = pS[psc[0] % NPS]
                        psc[0] += 1
                        S.op("pe", I("matmul", ps_[:, 0:nq], lhsT=kb_[:, kt * 128:(kt + 1) * 128],
                                     rhs=qb__[:, t0 * 128:t0 * 128 + nq], start=True, stop=True),
                             reads=[kb_, qb__], writes=[ps_])
                        return ps_

                    def emit_rest(ki, ps_, accs=accs, nq=nq, ntl=ntl):
                        kt, bd = keys[ki]
                        pt_ = PTs[ptc[0] % 4]
                        ptc[0] += 1
                        if bd is not None:
                            sbb = sbias[ptc[0] % 2]
                            bias_ap = m1[:, bd, :] if mixer == 1 else bb[:, bd, :]
                            bias_buf = m1 if mixer == 1 else bb
                            S.op("dve", I("scalar_tensor_tensor", out=sbb[:, 0:nq], in0=ps_[:, 0:nq], scalar=0.125,
                                          in1=bias_ap, op0=ALU.mult, op1=ALU.add), reads=[ps_, bias_buf], writes=[sbb])
                            S.op("act", I("activation", out=pt_[:, 0:nq], in_=sbb[:, 0:nq], func=AF.Exp),
                                 reads=[sbb], writes=[pt_])
                        else:
                            S.op("act", I("activation", out=pt_[:, 0:nq], in_=ps_[:, 0:nq], func=AF.Exp, scale=0.125),
                                 reads=[ps_], writes=[pt_])
                        for jq in range(ntl):
                            ab = accs[jq // (4 // nacc)]
                            S.op("pe", I("matmul", ab[:, jq % (4 // nacc), :], lhsT=pt_[:, jq * 128:(jq + 1) * 128],
                                         rhs=Vb[:, kt, :], start=False, stop=True, skip_group_check=True),
                                 reads=[pt_, Vb], writes=[ab], pe_acc=True, partial=True)

                    LOOK = 2
                    pend = []
                    for ki in range(len(keys)):
                        pend.append(emit_mm1(ki))
                        if ki >= LOOK:
                            emit_rest(ki - LOOK, pend[ki - LOOK])
                    for ki in range(max(0, len(keys) - LOOK), len(keys)):
                        emit_rest(ki, pend[ki])
                for jq in range(ntl):
                    t = t0 + jq
                    accv = [pA[aset * gsz + a][jq // (4 // nacc)][:, jq % (4 // nacc), :] for a in range(gsz)]
                    accb = [pA[aset * gsz + a][jq // (4 // nacc)] for a in range(gsz)]
                    if mixer == 0:
                        S.op("act", I("copy", out=o0[:, 0:dv1], in_=accv[0]), reads=[accb[0]], writes=[o0])
                        S.op("act", I("copy", out=o1[:, 0:dv1], in_=accv[1]), reads=[accb[1]], writes=[o1])
                        S.op("dve", I("reciprocal", out=rs_[:, 0:1], in_=o0[:, dv:dv1]), reads=[o0], writes=[rs_], partial=True)
                        S.op("dve", I("reciprocal", out=rs_[:, 1:2], in_=o1[:, dv:dv1]), reads=[o1, rs_], writes=[rs_], partial=True)
                        S.op("dve", I("tensor_scalar", out=rs_[:, 1:2], in0=rs_[:, 1:2], scalar1=nlam[:], scalar2=None, op0=ALU.mult),
                             reads=[rs_, nlam], writes=[rs_])
                        S.op("dve", I("tensor_scalar", out=o0[:, 0:dv], in0=o0[:, 0:dv], scalar1=rs_[:, 0:1], scalar2=None, op0=ALU.mult),
                             reads=[o0, rs_], writes=[o0])
                        S.op("dve", I("scalar_tensor_tensor", out=o0[:, 0:dv], in0=o1[:, 0:dv], scalar=rs_[:, 1:2], in1=o0[:, 0:dv],
                                                                     op0=ALU.mult, op1=ALU.add), reads=[o0, o1, rs_], writes=[o0])
                        S.op("act", I("activation", out=jk[:], in_=o0[:, 0:dv], func=AF.Square, accum_out=ss1[:]),
                             reads=[o0], writes=[jk, ss1])
                        S.op("dve", I("tensor_scalar", out=ss1[:], in0=ss1[:], scalar1=1.0 / dv, scalar2=EPS, op0=ALU.mult, op1=ALU.add),
                             reads=[ss1], writes=[ss1])
                        S.op("act", I("activation", out=ss1[:], in_=ss1[:], func=AF.Sqrt), reads=[ss1], writes=[ss1])
                        S.op("dve", I("reciprocal", out=ss1[:], in_=ss1[:]), reads=[ss1], writes=[ss1])
                        ast = aost[aoc[0] % 4]
                        aoc[0] += 1
                        S.op("dve", I("scalar_tensor_tensor", out=ast[:, 0:dv], in0=o0[:, 0:dv], scalar=ss1[:],
                                      in1=subg[:], op0=ALU.mult, op1=ALU.mult),
                             reads=[o0, ss1, subg], writes=[ast])
                        S.dma("pool", ao_d[t, :, g * dv:(g + 1) * dv], ast[:, 0:dv], reads=[ast], writes=[ao_d], partial=True)
                    else:
                        S.op("act", I("copy", out=o0[:, 0:dv1], in_=accv[0]), reads=[accb[0]], writes=[o0])
                        if mixer == 1:
                            S.op("dve", I("tensor_tensor", out=o0[:, dv:dv1], in0=o0[:, dv:dv1], in1=sk[:, g:g + 1], op=ALU.add),
                                 reads=[o0, sk], writes=[o0])
                        S.op("dve", I("reciprocal", out=rs_[:, 0:1], in_=o0[:, dv:dv1]), reads=[o0], writes=[rs_])
                        ast = aost[aoc[0] % 4]
                        aoc[0] += 1
                        S.op("dve", I("tensor_scalar", out=ast[:, 0:dv], in0=o0[:, 0:dv], scalar1=rs_[:, 0:1],
                                      scalar2=None, op0=ALU.mult), reads=[o0, rs_], writes=[ast])
                        S.dma("pool", ao_d[t, :, g * dv:(g + 1) * dv], ast[:, 0:dv], reads=[ast], writes=[ao_d], partial=True)
        S.phase_end()

        TP = T if need_ctx else TL
        S.phase_begin()
        H2T = S.sb([128, 8, TP * 128], BF16, "H2T")
        Wgt = S.sb([128, TP, NEXP], F32, "Wgt")
        S.phase_begin()
        wob = S.sb([128, 8, D], BF16, "wob")
        wst = [S.sb([128, 8, 512], F32, "wst") for _ in range(2)]
        for j in range(2):
            S.dma("sp" if j == 0 else "act", wst[j][:], wo[mixer][:, j * 512:(j + 1) * 512].rearrange("(c p) n -> p c n", p=128),
                  reads=[wo[mixer]], writes=[wst[j]])
            S.op("pool", I("tensor_copy", out=wob[:, :, j * 512:(j + 1) * 512], in_=wst[j][:]),
                 reads=[wst[j]], writes=[wob], partial=True)
        gbc = S.sb([128, D], F32, "gbc2")
        bc_load("sp", gbc, n2g, n2g[li:li + 1, :])
        G1 = [S.sb([128, D], F32, "G1") for _ in range(2)]
        A2 = [S.sb([128, D], F32, "A2") for _ in range(2)]
        B2 = [S.sb([128, D], F32, "B2") for _ in range(2)]
        for w_ in range(2):
            load_mod(w_, 2, G1[w_])
            load_mod(w_, 4, A2[w_], q="act")
            S.op("dve", I("scalar_tensor_tensor", out=A2[w_][:], in0=A2[w_][:], scalar=1.0, in1=gbc[:],
                                                                op0=ALU.add, op1=ALU.mult), reads=[A2[w_], gbc], writes=[A2[w_]])
            load_mod(w_, 3, B2[w_], q="act")
        wrt = S.sb([128, 8, 36], F32, "wrt")
        S.dma("sp", wrt[:], wr[li].rearrange("(c p) n -> p c n", p=128), reads=[wr], writes=[wrt])
        brt = S.sb([128, 36], F32, "brt")
        bc_load("sp", brt, br, br[li:li + 1, :])
        NB = 2
        aot = [S.sb([128, D], BF16, "aot") for _ in range(NB)]
        xts = [S.sb([128, D], F32, "xt4") for _ in range(NB)]
        aoT = [S.sb([128, 8, 128], BF16, "aoT") for _ in range(NB)]
        pT4 = [S.ps([128, 8, 128], BF16, "pT4") for _ in range(1)]
        py = [S.ps([128, 512], F32, "py") for _ in range(2)]
        ptf = S.ps([128, 8, 128], F32, "ptf")
        plg = S.ps([128, 36], F32, "plg")
        junk = S.sb([128, D], F32, "junk4")
        tmpf = S.sb([128, D], F32, "tmpf4")
        h2f = S.sb([128, D], F32, "h2f")
        h2b = S.sb([128, D], BF16, "h2b")
        h2T32 = S.sb([128, 8, 128], F32, "h2T32")
        rstd4 = [S.sb([128, 1], F32, "rstd4") for _ in range(NB)]
        lgt = S.sb([128, 36], F32, "lgt")
        sm = S.sb([128, 16], F32, "sm")
        ohg = S.sb([128, 4], F32, "ohg")
        lem = S.sb([128, 32], F32, "lem")
        top8 = S.sb([128, 8], F32, "top8")
        mk1 = S.sb([128, 32], F32, "mk1")
        mk2 = S.sb([128, 32], F32, "mk2")
        for t in range(TP):
            i = t % NB
            isctx = 1 if t >= TL else 0
            xt = xts[i]
            S.dma("sp", xt[:], xbuf[t], reads=[xbuf], writes=[xt])
            S.dma("act", aot[i][:], ao_d[t], reads=[ao_d], writes=[aot[i]])
            pT = pT4[0]
            for c in range(8):
                S.op("pe", I("transpose", out=pT[:, c, :], in_=aot[i][:, c * 128:(c + 1) * 128], identity=identb[:]),
                     reads=[aot[i], identb], writes=[pT], partial=True, pe_acc=True)
            S.op("act", I("copy", out=aoT[i][:], in_=pT[:]), reads=[pT], writes=[aoT[i]])
            for j in range(2):
                for c in range(8):
                    S.op("pe", I("matmul", py[j][:], lhsT=aoT[i][:, c, :], rhs=wob[:, c, j * 512:(j + 1) * 512],
                                                                start=(c == 0), stop=(c == 7)),
                         reads=[aoT[i], wob], writes=[py[j]], pe_acc=(c > 0))
                S.op("dve", I("tensor_tensor", out=tmpf[:, j * 512:(j + 1) * 512], in0=py[j][:],
                                                                        in1=G1[isctx][:, j * 512:(j + 1) * 512], op=ALU.mult),
                     reads=[py[j], G1[isctx]], writes=[tmpf], partial=True)
            S.op("dve", I("tensor_tensor", out=xt[:], in0=xt[:], in1=tmpf[:], op=ALU.add), reads=[xt, tmpf], writes=[xt])
            S.dma("pool", xbuf[t], xt[:], reads=[xt], writes=[xbuf], partial=True)
            rms_rstd(xt[:], xt, rstd4[i], junk, D)
            S.op("dve", I("scalar_tensor_tensor", out=tmpf[:], in0=xt[:], scalar=rstd4[i][:], in1=A2[isctx][:],
                                                                               op0=ALU.mult, op1=ALU.mult),
                 reads=[xt, rstd4[i], A2[isctx]], writes=[tmpf])
            S.op("dve", I("tensor_tensor", out=h2f[:], in0=tmpf[:], in1=B2[isctx][:], op=ALU.add),
                 reads=[tmpf, B2[isctx]], writes=[h2f])
            S.op("pool", I("tensor_copy", out=h2b[:], in_=h2f[:]), reads=[h2f], writes=[h2b])
            for c in range(8):
                S.op("pe", I("transpose", out=pT[:, c, :], in_=h2b[:, c * 128:(c + 1) * 128], identity=identb[:]),
                     reads=[h2b, identb], writes=[pT], partial=True, pe_acc=True)
            S.op("act", I("copy", out=H2T[:, :, t * 128:(t + 1) * 128], in_=pT[:]), reads=[pT], writes=[H2T], partial=True)
            for c in range(8):
                S.op("pe", I("transpose", out=ptf[:, c, :], in_=h2f[:, c * 128:(c + 1) * 128], identity=identf[:]),
                     reads=[h2f, identf], writes=[ptf], partial=True, pe_acc=True)
            S.op("act", I("copy", out=h2T32[:], in_=ptf[:]), reads=[ptf], writes=[h2T32])
            for c in range(8):
                S.op("pe", I("matmul", plg[:], lhsT=h2T32[:, c, :], rhs=wrt[:, c, :], start=(c == 0), stop=(c == 7)),
                     reads=[h2T32, wrt], writes=[plg], pe_acc=(c > 0))
            S.op("dve", I("tensor_tensor", out=lgt[:], in0=plg[:], in1=brt[:], op=ALU.add), reads=[plg, brt], writes=[lgt])
            S.op("dve", I("tensor_reduce", out=sm[:, 0:1], in_=lgt[:, 0:4], axis=AX.X, op=ALU.max), reads=[lgt], writes=[sm])
            S.op("dve", I("tensor_scalar", out=ohg[:], in0=lgt[:, 0:4], scalar1=sm[:, 0:1], scalar2=None, op0=ALU.is_equal),
                 reads=[lgt, sm], writes=[ohg])
            S.op("dve", I("tensor_scalar", out=sm[:, 1:2], in0=sm[:, 0:1], scalar1=-1.0, scalar2=None, op0=ALU.mult),
                 reads=[sm], writes=[sm])
            S.op("act", I("activation", out=mk1[:, 0:4], in_=lgt[:, 0:4], func=AF.Exp, bias=sm[:, 1:2], accum_out=sm[:, 2:3]),
                 reads=[lgt, sm], writes=[mk1, sm])
            S.op("dve", I("reciprocal", out=sm[:, 3:4], in_=sm[:, 2:3]), reads=[sm], writes=[sm])
            S.op("dve", I("tensor_scalar", out=ohg[:], in0=ohg[:], scalar1=-1.0, scalar2=1e30, op0=ALU.add, op1=ALU.mult),
                 reads=[ohg], writes=[ohg])
            S.op("dve", I("tensor_tensor", out=lem[:].rearrange("p (g e) -> p g e", e=8),
                                                  in0=lgt[:, 4:36].rearrange("p (g e) -> p g e", e=8),
                                                  in1=ohg[:].unsqueeze(2).broadcast_to([128, 4, 8]), op=ALU.add),
                 reads=[lgt, ohg], writes=[lem])
            S.op("dve", I("max", out=top8[:], in_=lem[:]), reads=[lem], writes=[top8])
            S.op("dve", I("tensor_scalar", out=mk1[:], in0=lem[:], scalar1=top8[:, 0:1], scalar2=None, op0=ALU.is_equal),
                 reads=[lem, top8], writes=[mk1])
            S.op("dve", I("tensor_scalar", out=mk2[:], in0=lem[:], scalar1=top8[:, 1:2], scalar2=None, op0=ALU.is_equal),
                 reads=[lem, top8], writes=[mk2])
            S.op("dve", I("tensor_tensor", out=sm[:, 4:5], in0=top8[:, 1:2], in1=top8[:, 0:1], op=ALU.subtract),
                 reads=[top8, sm], writes=[sm])
            S.op("act", I("activation", out=sm[:, 5:6], in_=sm[:, 4:5], func=AF.Exp), reads=[sm], writes=[sm])
            S.op("dve", I("tensor_scalar", out=sm[:, 6:7], in0=sm[:, 5:6], scalar1=1.0, scalar2=None, op0=ALU.add),
                 reads=[sm], writes=[sm])
            S.op("dve", I("reciprocal", out=sm[:, 7:8], in_=sm[:, 6:7]), reads=[sm], writes=[sm])
            S.op("dve", I("tensor_tensor", out=sm[:, 8:9], in0=sm[:, 7:8], in1=sm[:, 3:4], op=ALU.mult), reads=[sm], writes=[sm])
            S.op("dve", I("tensor_tensor", out=sm[:, 9:10], in0=sm[:, 8:9], in1=sm[:, 5:6], op=ALU.mult), reads=[sm], writes=[sm])
            S.op("dve", I("tensor_scalar", out=mk1[:], in0=mk1[:], scalar1=sm[:, 8:9], scalar2=None, op0=ALU.mult),
                 reads=[mk1, sm], writes=[mk1])
            S.op("dve", I("scalar_tensor_tensor", out=Wgt[:, t, :], in0=mk2[:], scalar=sm[:, 9:10], in1=mk1[:],
                                                              op0=ALU.mult, op1=ALU.add),
                 reads=[mk2, sm, mk1], writes=[Wgt], partial=True)
        S.phase_end()

        S.phase_begin()
        xts = [S.sb([128, D], F32, "xt5") for _ in range(NB)]
        tmpf = S.sb([128, D], F32, "tmpf5")
        junk = tmpf
        rstd4 = [S.sb([128, 1], F32, "rstd5") for _ in range(NB)]
        acc = S.sb([128, TP, D], F32, "acc")
        wgb = S.sb([128, 8, DEXP], BF16, "wgb")
        wub = S.sb([128, 8, DEXP], BF16, "wub")
        wdb = S.sb([128, 4, D], BF16, "wdb")
        st5 = [S.sb([128, 8, 512], F32, "st5") for _ in range(2)]
        pg = [S.ps([128, 512], F32, "pg") for _ in range(2)]
        pu = [S.ps([128, 512], F32, "pu") for _ in range(2)]
        py5 = [S.ps([128, 512], F32, "py5") for _ in range(2)]
        sgs = [S.sb([128, 512], BF16, "sg") for _ in range(2)]
        hid = [S.sb([128, 4, 512], BF16, "hid") for _ in range(2)]
        stc = [0]
        for ex in range(NEXP):
            for (src, dst, shp) in ((wg, wgb, 0), (wu, wub, 0), (wd, wdb, 1)):
                s_ = st5[stc[0] % 2]
                stc[0] += 1
                if shp == 0:
                    S.dma("sp" if stc[0] % 2 else "act", s_[:], src[li, ex].rearrange("(c p) n -> p c n", p=128), reads=[src], writes=[s_])
                    S.op("pool", I("tensor_copy", out=dst[:], in_=s_[:]), reads=[s_], writes=[dst])
                else:
                    S.dma("sp" if stc[0] % 2 else "act", s_[:].rearrange("p c n -> p (c n)").rearrange("p (c n) -> p c n", c=4),
                          src[li, ex].rearrange("(c p) n -> p c n", p=128), reads=[src], writes=[s_])
                    S.op("pool", I("tensor_copy", out=dst[:], in_=s_[:].rearrange("p c n -> p (c n)").rearrange("p (c n) -> p c n", c=4)),
                         reads=[s_], writes=[dst])
            nchunk = (TP + 3) // 4
            for ch in range(nchunk):
                t0 = ch * 4
                ntl = min(4, TP - t0)
                nq = ntl * 128
                hd_ = hid[ch % 2]
                for hb_ in range(4):
                    for c in range(8):
                        S.op("pe", I("matmul", pg[hb_ % 2][:, 0:nq], lhsT=wgb[:, c, hb_ * 128:(hb_ + 1) * 128],
                                                                                 rhs=H2T[:, c, t0 * 128:t0 * 128 + nq], start=(c == 0), stop=(c == 7)),
                             reads=[wgb, H2T], writes=[pg[hb_ % 2]], pe_acc=(c > 0))
                    for c in range(8):
                        S.op("pe", I("matmul", pu[hb_ % 2][:, 0:nq], lhsT=wub[:, c, hb_ * 128:(hb_ + 1) * 128],
                                                                                 rhs=H2T[:, c, t0 * 128:t0 * 128 + nq], start=(c == 0), stop=(c == 7)),
                             reads=[wub, H2T], writes=[pu[hb_ % 2]], pe_acc=(c > 0))
                    sg_ = sgs[hb_ % 2]
                    S.op("act", I("activation", out=sg_[:, 0:nq], in_=pg[hb_ % 2][:, 0:nq], func=AF.Silu),
                         reads=[pg[hb_ % 2]], writes=[sg_])
                    S.op("dve", I("tensor_tensor", out=hd_[:, hb_, 0:nq], in0=sg_[:, 0:nq], in1=pu[hb_ % 2][:, 0:nq], op=ALU.mult),
                         reads=[sg_, pu[hb_ % 2]], writes=[hd_], partial=True)
                for jq in range(ntl):
                    t = t0 + jq
                    for j in range(2):
                        for hb_ in range(4):
                            S.op("pe", I("matmul", py5[j][:], lhsT=hd_[:, hb_, jq * 128:(jq + 1) * 128],
                                                                                      rhs=wdb[:, hb_, j * 512:(j + 1) * 512], start=(hb_ == 0), stop=(hb_ == 3)),
                                 reads=[hd_, wdb], writes=[py5[j]], pe_acc=(hb_ > 0))
                        if ex == 0:
                            S.op("dve", I("tensor_scalar", out=acc[:, t, j * 512:(j + 1) * 512], in0=py5[j][:], scalar1=Wgt[:, t, ex:ex + 1],
                                                                                scalar2=None, op0=ALU.mult), reads=[py5[j], Wgt], writes=[acc], partial=True)
                        else:
                            S.op("dve", I("scalar_tensor_tensor", out=acc[:, t, j * 512:(j + 1) * 512], in0=py5[j][:], scalar=Wgt[:, t, ex:ex + 1],
                                                                                       in1=acc[:, t, j * 512:(j + 1) * 512], op0=ALU.mult, op1=ALU.add),
                                 reads=[py5[j], Wgt, acc], writes=[acc], partial=True)
        G2b = S.sb([128, D], F32, "G2")
        G2 = [G2b, G2b]
        load_mod(0, 5, G2b)
        last = li == cfg.depth - 1
        if last:
            fg = S.sb([128, D], F32, "fg")
            bc_load("sp", fg, fing, fing[0:1, :])
        for t in range(TP):
            i = t % NB
            isctx = 1 if t >= TL else 0
            xt = xts[i]
            if t == TL:
                load_mod(1, 5, G2b)
            S.dma("sp", xt[:], xbuf[t], reads=[xbuf], writes=[xt])
            S.op("dve", I("tensor_tensor", out=tmpf[:], in0=acc[:, t, :], in1=G2[isctx][:], op=ALU.mult),
                 reads=[acc, G2[isctx]], writes=[tmpf])
            S.op("dve", I("tensor_tensor", out=xt[:], in0=xt[:], in1=tmpf[:], op=ALU.add), reads=[xt, tmpf], writes=[xt])
            if not last:
                S.dma("pool", xbuf[t], xt[:], reads=[xt], writes=[xbuf], partial=True)
            else:
                rms_rstd(xt[:], xt, rstd4[i], junk, D)
                S.op("dve", I("scalar_tensor_tensor", out=xt[:], in0=xt[:], scalar=rstd4[i][:], in1=fg[:], op0=ALU.mult, op1=ALU.mult),
                     reads=[xt, rstd4[i], fg], writes=[xt])
                S.dma("pool", out[t], xt[:], reads=[xt], writes=[out], partial=True, is_output=True)
        S.phase_end()
        S.phase_end()
    S.finish()
    return nc, S


def _rope_tables(cfg, core):
    TL, T = cfg.TL, cfg.T
    tok = core * TL * 128 + np.arange(TL * 128)
    pos = np.stack([tok // GRID_W, tok % GRID_W], axis=-1).astype(np.float32)
    quarter = HD // 4
    inv_freq = (1.0 / (10000.0 ** (np.arange(quarter, dtype=np.float32) / quarter))).astype(np.float32)
    ang = pos[:, :, None] * inv_freq
    cos = np.cos(ang).astype(np.float32)
    sin = np.sin(ang).astype(np.float32)
    cosf = np.ones((T * 128, 2, 2, 16), np.float32)
    sinf = np.zeros((T * 128, 2, 2, 16), np.float32)
    cosf[:TL * 128] = cos[:, :, None, :]
    sinf[:TL * 128] = sin[:, :, None, :]
    return cosf.reshape(T, 128, HD), sinf.reshape(T, 128, HD)


def _bias3_table(cfg, core, rpb):
    TL, rows = cfg.TL, cfg.rows
    kh = min(8, rows)
    slots_local = [0, 1, TL - 2, TL - 1, 2]
    outp = np.full((5, 16, 128, 5, 128), NEG, np.float32)
    qq = np.arange(128)
    kk = np.arange(128)
    for s, jl in enumerate(slots_local):
        J = core * TL + jl
        qr = 2 * J + qq // 64
        qc = qq % 64
        r0 = np.clip(qr - kh // 2, 0, rows - kh)
        c0 = np.clip(qc - 8, 0, GRID_W - 16)
        for d in range(5):
            Jk = J + d - 2
            if Jk < 0 or 2 * Jk >= rows:
                continue
            kr = 2 * Jk + kk // 64
            kc = kk % 64
            valid = ((kr[:, None] >= r0[None, :]) & (kr[:, None] < r0[None, :] + kh) &
                     (kc[:, None] >= c0[None, :]) & (kc[:, None] < c0[None, :] + 16))
            dr = np.clip(kr[:, None] - qr[None, :] + 7, 0, 14)
            dc = np.clip(kc[:, None] - qc[None, :] + 15, 0, 30)
            vals = rpb[:, dr, dc]
            outp[s, :, :, d, :] = np.where(valid[None], vals, np.float32(NEG))
    return outp


def _mask1():
    kk = np.arange(128)[:, None]
    qi = np.arange(128)[None, :]
    m = np.zeros((128, 3, 128), np.float32)
    m[:, 0, :] = np.where(kk >= qi, 0.0, NEG)
    m[:, 2, :] = np.where(kk <= qi, 0.0, NEG)
    return m


def make_in_maps(cfg, inp):
    ncore, TL, T = cfg.ncore, cfg.TL, cfg.T
    f = lambda a: np.ascontiguousarray(np.asarray(a, dtype=np.float32))
    x = f(inp["x"])[0]
    ctx = f(inp["ctx"])[0]
    shared = {
        "cc": np.stack([f(inp["c"])[0], f(inp["c_ctx"])], 0),
        "mod_w": f(inp["mod_w"]), "mod_b": f(inp["mod_b"]),
        "n1g": f(inp["norm1_g"]), "n2g": f(inp["norm2_g"]), "fing": f(inp["final_g"])[None, :],
        "wqkv0": f(inp["a_w_qkv"])[0], "wqkv1": f(inp["b_w_qkv"])[0], "wqkv2": f(inp["c_w_qkv"])[0], "wqkv3": f(inp["d_w_qkv"])[0],
        "wo0": f(inp["a_w_o"])[0], "wo1": f(inp["b_w_o"])[0], "wo2": f(inp["c_w_o"])[0], "wo3": f(inp["d_w_o"])[0],
        "a_lam": f(inp["a_lam"])[0], "a_sub": f(inp["a_subln_g"]), "b_sink": f(inp["b_sink"]), "c_qkg": f(inp["c_qk_norm_g"])[0],
        "mask1": _mask1(),
        "wr": np.ascontiguousarray(np.concatenate([f(inp["moe_router_g"]), f(inp["moe_router_e"])], axis=-1)),
        "br": np.ascontiguousarray(np.concatenate([f(inp["moe_router_g_b"]), f(inp["moe_router_e_b"])], axis=-1)),
        "moe_wg": f(inp["moe_w_gate"]), "moe_wu": f(inp["moe_w_up"]), "moe_wd": f(inp["moe_w_down"]),
    }
    rpb = f(inp["d_rpb"])[0]
    maps = []
    for c in range(ncore):
        m = dict(shared)
        xi = np.concatenate([x[c * TL * 128:(c + 1) * TL * 128], ctx], 0).reshape(T, 128, D)
        m["xin"] = np.ascontiguousarray(xi)
        cosf, sinf = _rope_tables(cfg, c)
        m["cosT"], m["sinT"] = cosf, sinf
        sel = np.zeros((128, 2 * ncore), np.float32)
        if c > 0:
            sel[:, c - 1] = 1.0
        if c < ncore - 1:
            sel[:, ncore + c + 1] = 1.0
        m["selT"] = sel
        m["bias3"] = _bias3_table(cfg, c, rpb)
        maps.append(m)
    return maps


_CACHE = {}


def kernel(**inputs):
    cfg = Cfg()
    if "nc" not in _CACHE:
        _CACHE["nc"] = build(cfg)[0]
    nc = _CACHE["nc"]
    in_maps = make_in_maps(cfg, inputs)
    res = run_bass_kernel_spmd(nc, in_maps, core_ids=list(range(cfg.ncore)))
    outs = [np.asarray(r["out"], dtype=np.float32).reshape(cfg.TL * 128, D) for r in res.results]
    return np.concatenate(outs, 0)[None].astype(np.float32)
```
